# Optimizing a Trainium2 kernel written in Bass

```python
import jax, jax.numpy as jnp
from jax import lax
import numpy as np

D_MODEL = 1024
BATCH = 4
SEQ = 4096
DEPTH = 1

CHUNK = 64
Q_BLOCK = 128
CONV_DIM = D_MODEL
CONV_WIDTH = 3
N_HEADS = 8
QK_NOPE_DIM = 128
QK_ROPE_DIM = 64
V_HEAD_DIM = 128
QK_HEAD_DIM = QK_NOPE_DIM + QK_ROPE_DIM
Q_LORA_RANK = 384
KV_LORA_RANK = 256
ROPE_BASE = 10000.0
N_EXPERTS = 32
TOP_K = 4
D_FF_EXPERT = D_MODEL
SWIGLU_LIMIT = 7.0
SWIGLU_ALPHA = 1.702
MOE_BLOCK = 128
LN_EPS = 1e-5
RMS_EPS = 1e-6
DEEPNORM_ALPHA = (2 * DEPTH) ** 0.25
DEEPNORM_BETA = (8 * DEPTH) ** -0.25
IN_SPLITS = (CONV_DIM, CONV_DIM, CONV_DIM, Q_LORA_RANK, KV_LORA_RANK, QK_ROPE_DIM, D_MODEL, D_MODEL)
IN_DIM = sum(IN_SPLITS)

kernel_name = "hybrid_conv_mla_moe_deepnorm_block"


def layer_norm(u, g, b):
    uf = u.astype(jnp.float32)
    mu = jnp.mean(uf, axis=-1, keepdims=True)
    var = jnp.mean(jnp.square(uf - mu), axis=-1, keepdims=True)
    return ((uf - mu) * lax.rsqrt(var + LN_EPS) * g.astype(jnp.float32) + b.astype(jnp.float32)).astype(u.dtype)


def rms_norm(u, g):
    uf = u.astype(jnp.float32)
    ms = jnp.mean(jnp.square(uf), axis=-1, keepdims=True)
    return (uf * lax.rsqrt(ms + RMS_EPS) * g.astype(jnp.float32)).astype(u.dtype)


def rope_tables(positions, dtype):
    inv_freq = ROPE_BASE ** (-jnp.arange(0, QK_ROPE_DIM, 2, dtype=jnp.float32) / QK_ROPE_DIM)
    ang = positions.astype(jnp.float32)[..., None] * inv_freq
    return jnp.cos(ang).astype(dtype), jnp.sin(ang).astype(dtype)


def apply_rope(u, cos, sin):
    half = u.shape[-1] // 2
    u1, u2 = u[..., :half], u[..., half:]
    return jnp.concatenate([u1 * cos - u2 * sin, u1 * sin + u2 * cos], axis=-1)


def causal_depthwise_conv(u, w):
    c = u.shape[-1]
    return lax.conv_general_dilated(u, w[:, None, :].astype(u.dtype), window_strides=(1,),
                                    padding=[(CONV_WIDTH - 1, 0)],
                                    dimension_numbers=('NWC', 'WIO', 'NWC'),
                                    feature_group_count=c)


def chunk_causal_attention(q, k, v):
    b, s, h, dqk = q.shape
    nqb = s // Q_BLOCK
    qb = q.reshape(b, nqb, Q_BLOCK, h, dqk).transpose(1, 0, 2, 3, 4)
    key_chunk = jnp.arange(s) // CHUNK
    scale = dqk ** -0.5

    def one_block(args):
        q_blk, blk = args
        q_chunk = (blk * Q_BLOCK + jnp.arange(Q_BLOCK)) // CHUNK
        allowed = key_chunk[None, :] <= q_chunk[:, None]
        scores = jnp.einsum('bqhd,bkhd->bhqk', q_blk, k).astype(jnp.float32) * scale
        scores = jnp.where(allowed[None, None], scores, -jnp.inf)
        probs = jax.nn.softmax(scores, axis=-1).astype(v.dtype)
        return jnp.einsum('bhqk,bkhd->bqhd', probs, v)

    out = lax.map(one_block, (qb, jnp.arange(nqb)))
    return out.transpose(1, 0, 2, 3, 4).reshape(b, s, h, v.shape[-1])


def hybrid_mixer(x, cos, sin, w_in, conv_w, q_norm_g, w_uq, kv_norm_g, w_uk, w_uv,
                 w_conv_branch, w_attn_branch, w_out):
    b, s, _ = x.shape
    proj = x @ w_in
    offs = [int(o) for o in np.cumsum(IN_SPLITS)[:-1]]
    gb, gc, hc, q_lat, kv_lat, k_pe, g_conv, g_attn = jnp.split(proj, offs, axis=-1)
    y_conv = (gb * causal_depthwise_conv(gc * hc, conv_w)) @ w_conv_branch
    q = (rms_norm(q_lat, q_norm_g) @ w_uq).reshape(b, s, N_HEADS, QK_HEAD_DIM)
    q_nope, q_pe = q[..., :QK_NOPE_DIM], q[..., QK_NOPE_DIM:]
    q_pe = apply_rope(q_pe, cos[:, :, None, :], sin[:, :, None, :])
    ckv = rms_norm(kv_lat, kv_norm_g)
    k_nope = (ckv @ w_uk).reshape(b, s, N_HEADS, QK_NOPE_DIM)
    v = (ckv @ w_uv).reshape(b, s, N_HEADS, V_HEAD_DIM)
    k_pe = jnp.broadcast_to(apply_rope(k_pe, cos, sin)[:, :, None, :], (b, s, N_HEADS, QK_ROPE_DIM))
    q_full = jnp.concatenate([q_nope, q_pe], axis=-1)
    k_full = jnp.concatenate([k_nope, k_pe], axis=-1)
    attn = chunk_causal_attention(q_full, k_full, v).reshape(b, s, N_HEADS * V_HEAD_DIM)
    y_attn = attn @ w_attn_branch
    merged = jax.nn.sigmoid(g_conv) * y_conv + jax.nn.sigmoid(g_attn) * y_attn
    return merged @ w_out


def moe_ffn(x2d, w_router, b_router, w_gate_up, b_gate_up, w_down, b_down):
    t, d = x2d.shape
    n_assign = t * TOP_K
    logits = (x2d @ w_router + b_router).astype(jnp.float32)
    top_logits, top_idx = lax.top_k(logits, TOP_K)
    top_w = jax.nn.softmax(top_logits, axis=-1)
    flat_e = top_idx.reshape(-1)
    flat_tok = jnp.arange(n_assign, dtype=jnp.int32) // TOP_K
    flat_w = top_w.reshape(-1)
    order = jnp.argsort(flat_e)
    sorted_e = flat_e[order]
    counts = jnp.bincount(flat_e, length=N_EXPERTS)
    start = jnp.cumsum(counts) - counts
    padded = (counts + MOE_BLOCK - 1) // MOE_BLOCK * MOE_BLOCK
    padded_end = jnp.cumsum(padded)
    padded_start = padded_end - padded
    dest = padded_start[sorted_e] + (jnp.arange(n_assign, dtype=jnp.int32) - start[sorted_e])
    n_blocks = n_assign // MOE_BLOCK + N_EXPERTS
    n_slots = n_blocks * MOE_BLOCK
    slot_tok = jnp.zeros((n_slots,), jnp.int32).at[dest].set(flat_tok[order])
    slot_w = jnp.zeros((n_slots,), jnp.float32).at[dest].set(flat_w[order])
    block_e = jnp.minimum(jnp.searchsorted(padded_end, jnp.arange(n_blocks) * MOE_BLOCK, side='right'),
                          N_EXPERTS - 1)

    def expert_block(args):
        tok, e = args
        xb = x2d[tok]
        gu = xb @ w_gate_up[e] + b_gate_up[e]
        gate = jnp.minimum(gu[:, :D_FF_EXPERT], SWIGLU_LIMIT)
        up = jnp.clip(gu[:, D_FF_EXPERT:], -SWIGLU_LIMIT, SWIGLU_LIMIT)
        hid = (up + 1) * (gate * jax.nn.sigmoid(SWIGLU_ALPHA * gate))
        return hid @ w_down[e] + b_down[e]

    out = lax.map(expert_block, (slot_tok.reshape(n_blocks, MOE_BLOCK), block_e))
    out = out.reshape(n_slots, d) * slot_w[:, None].astype(out.dtype)
    return jnp.zeros_like(x2d).at[slot_tok].add(out)


def setup_inputs(seed: int = 0) -> dict:
    key = jax.random.key(seed)
    ks = jax.random.split(key, 24)
    L = DEPTH
    beta = DEEPNORM_BETA

    def nrm(k, shape, scale):
        return jax.random.normal(k, shape, jnp.float32) * scale

    def gain(k, dim):
        return jnp.ones((L, dim), jnp.float32) + 0.01 * jax.random.normal(k, (L, dim), jnp.float32)

    x = jax.random.normal(ks[0], (BATCH, SEQ, D_MODEL), jnp.float32)
    offset = jax.random.randint(ks[1], (BATCH, 1), 0, 8192, dtype=jnp.int32)
    positions = offset + jnp.arange(SEQ, dtype=jnp.int32)[None, :]
    return {
        "x": x,
        "positions": positions,
        "w_in": nrm(ks[2], (L, D_MODEL, IN_DIM), D_MODEL ** -0.5),
        "conv_w": nrm(ks[3], (L, CONV_WIDTH, CONV_DIM), CONV_WIDTH ** -0.5),
        "q_norm_g": gain(ks[4], Q_LORA_RANK),
        "w_uq": nrm(ks[5], (L, Q_LORA_RANK, N_HEADS * QK_HEAD_DIM), Q_LORA_RANK ** -0.5),
        "kv_norm_g": gain(ks[6], KV_LORA_RANK),
        "w_uk": nrm(ks[7], (L, KV_LORA_RANK, N_HEADS * QK_NOPE_DIM), KV_LORA_RANK ** -0.5),
        "w_uv": nrm(ks[8], (L, KV_LORA_RANK, N_HEADS * V_HEAD_DIM), beta * KV_LORA_RANK ** -0.5),
        "w_conv_branch": nrm(ks[9], (L, CONV_DIM, D_MODEL), beta * CONV_DIM ** -0.5),
        "w_attn_branch": nrm(ks[10], (L, N_HEADS * V_HEAD_DIM, D_MODEL), beta * (N_HEADS * V_HEAD_DIM) ** -0.5),
        "w_out": nrm(ks[11], (L, D_MODEL, D_MODEL), beta * D_MODEL ** -0.5),
        "ln1_g": gain(ks[12], D_MODEL),
        "ln1_b": nrm(ks[13], (L, D_MODEL), 0.01),
        "w_router": nrm(ks[14], (L, D_MODEL, N_EXPERTS), D_MODEL ** -0.5),
        "b_router": nrm(ks[15], (L, N_EXPERTS), 0.01),
        "w_gate_up": nrm(ks[16], (L, N_EXPERTS, D_MODEL, 2 * D_FF_EXPERT), beta * D_MODEL ** -0.5),
        "b_gate_up": nrm(ks[17], (L, N_EXPERTS, 2 * D_FF_EXPERT), 0.01),
        "w_down": nrm(ks[18], (L, N_EXPERTS, D_FF_EXPERT, D_MODEL), beta * D_FF_EXPERT ** -0.5),
        "b_down": nrm(ks[19], (L, N_EXPERTS, D_MODEL), 0.01),
        "ln2_g": gain(ks[20], D_MODEL),
        "ln2_b": nrm(ks[21], (L, D_MODEL), 0.01),
    }


def reference(x, positions, w_in, conv_w, q_norm_g, w_uq, kv_norm_g, w_uk, w_uv,
              w_conv_branch, w_attn_branch, w_out, ln1_g, ln1_b, w_router, b_router,
              w_gate_up, b_gate_up, w_down, b_down, ln2_g, ln2_b):
    b, s, d = x.shape
    cos, sin = rope_tables(positions, x.dtype)
    for l in range(DEPTH):
        mix = hybrid_mixer(x, cos, sin, w_in[l], conv_w[l], q_norm_g[l], w_uq[l], kv_norm_g[l],
                           w_uk[l], w_uv[l], w_conv_branch[l], w_attn_branch[l], w_out[l])
        x = layer_norm(DEEPNORM_ALPHA * x + mix, ln1_g[l], ln1_b[l])
        ffn = moe_ffn(x.reshape(b * s, d), w_router[l], b_router[l], w_gate_up[l], b_gate_up[l],
                      w_down[l], b_down[l]).reshape(b, s, d)
        x = layer_norm(DEEPNORM_ALPHA * x + ffn, ln2_g[l], ln2_b[l])
    return x
```

```python
from contextlib import ExitStack
import numpy as np
import concourse.bass as bass
import concourse.mybir as mybir
from concourse.bass_utils import run_bass_kernel_spmd

F32 = mybir.dt.float32
BF16 = mybir.dt.bfloat16
I32 = mybir.dt.int32
U32 = mybir.dt.uint32
AF = mybir.ActivationFunctionType
ALU = mybir.AluOpType
AX = mybir.AxisListType

NCORES = 8
D = 1024
T = 2048
SEG = 512
NSEG = 4
NE = 32
CAP = 384
NSLOT = NE * CAP
ALPHA = float(2.0 ** 0.25)
LN_EPS = 1e-5
RMS_EPS = 1e-6
QK_SCALE = float(192.0 ** -0.5)
MAGIC = 12582912.0
TWO_PI_S = float(2 * np.pi * (1 - 1e-6))

C_ID, C_ONE, C_MOWN, C_MOTH, C_TRI, C_IOTA, C_INVF, C_SGN, C_END = 0, 128, 256, 384, 512, 640, 672, 673, 674


class Ev:
    __slots__ = ("sem", "val", "eng")

    def __init__(self, sem, val, eng):
        self.sem, self.val, self.eng = sem, val, eng


class Buf:
    __slots__ = ("name", "w", "r")

    def __init__(self, name=""):
        self.name, self.w, self.r = name, None, {}


class Sched:
    SEM_LIMIT = 30000

    def __init__(self, nc, stack, n_dma_sems=64):
        self.nc, self.stack = nc, stack
        self.engs = {"pe": nc.tensor, "dve": nc.vector, "act": nc.scalar, "pool": nc.gpsimd, "sp": nc.sync}
        self.sem, self.cnt, self.waited, self.pending, self.last = {}, {}, {}, {}, {}
        self.nsem = 0
        for k in self.engs:
            self.sem[k] = self._newsem(k)
            self.cnt[k] = 0
            self.waited[k] = {}
            self.pending[k] = []
            self.last[k] = None
        self.dma_sems = [self._newsem("dma") for _ in range(n_dma_sems)]
        self.dma_cnt = [0] * n_dma_sems
        self.dma_last = [None] * n_dma_sems
        self.n_sw = (n_dma_sems * 5) // 8
        self.dma_i = {"sw": 0, "hw": 0}
        self.out_events = []

    def _newsem(self, tag):
        self.nsem += 1
        return self.stack.enter_context(self.nc.semaphore(f"s_{tag}_{self.nsem}"))

    def _wait(self, e, ev):
        if ev is None:
            return
        if ev.eng == e and e == "pe":
            return
        if ev.val is None:
            raise RuntimeError("waiting on unsignalled event")
        w = self.waited[e]
        key = id(ev.sem)
        if w.get(key, 0) >= ev.val:
            return
        self.engs[e].wait_ge(ev.sem, ev.val)
        w[key] = ev.val

    def _deps(self, e, reads, writes):
        for b in reads:
            self._wait(e, b.w)
        for b in writes:
            self._wait(e, b.w)
            for r in list(b.r.values()):
                if isinstance(r, list):
                    for x in r:
                        self._wait(e, x)
                else:
                    self._wait(e, r)

    def _mark(self, ev, reads, writes):
        for b in writes:
            b.w = ev
            b.r = {}
        for b in reads:
            if b not in writes:
                if ev.eng == "dma":
                    b.r.setdefault("dma", []).append(ev)
                else:
                    b.r[ev.eng] = ev

    def op(self, e, fn, reads=(), writes=(), signal=True):
        self._deps(e, reads, writes)
        ins = fn(self.engs[e])
        ev = Ev(None, None, e)
        if signal:
            if self.cnt[e] >= self.SEM_LIMIT:
                self.sem[e] = self._newsem(e)
                self.cnt[e] = 0
            self.cnt[e] += 1
            ins.then_inc(self.sem[e], 1)
            ev.sem, ev.val = self.sem[e], self.cnt[e]
            for p in self.pending[e]:
                p.sem, p.val = ev.sem, ev.val
            self.pending[e] = []
            self.last[e] = ev
        else:
            self.pending[e].append(ev)
        self._mark(ev, reads, writes)
        return ev

    def dma(self, q, fn, reads=(), writes=(), is_output=False):
        self._deps(q, reads, writes)
        if q == "pool":
            i = self.dma_i["sw"]
            self.dma_i["sw"] = (i + 1) % self.n_sw
        else:
            i = self.n_sw + self.dma_i["hw"]
            self.dma_i["hw"] = (self.dma_i["hw"] + 1) % (len(self.dma_sems) - self.n_sw)
        prev = self.dma_last[i]
        if prev is not None:
            w = self.waited[q]
            if w.get(id(prev.sem), 0) < prev.val:
                self.engs[q].wait_ge(prev.sem, prev.val)
                w[id(prev.sem)] = prev.val
        ins = fn(self.engs[q])
        self.dma_cnt[i] += 16
        ins.then_inc(self.dma_sems[i], 16)
        ev = Ev(self.dma_sems[i], self.dma_cnt[i], "dma")
        self.dma_last[i] = ev
        self._mark(ev, reads, writes)
        if is_output:
            self.out_events.append(ev)
        return ev

    def barrier(self):
        evs = [v for v in self.last.values() if v is not None] + [d for d in self.dma_last if d is not None]
        for e in self.engs:
            if self.pending[e]:
                raise RuntimeError("barrier with unsignalled instructions on " + e)
            for ev in evs:
                if ev.eng != e:
                    self._wait(e, ev)

    def finish(self):
        for ev in self.out_events:
            self._wait("sp", ev)
        for ev in self.dma_last:
            if ev is not None:
                self._wait("sp", ev)


def build_program(stage=9):
    nc = bass.Bass("TRN2", target_bir_lowering=False)

    def din(name, shape, dt=F32):
        return nc.dram_tensor(name, list(shape), dt, kind="ExternalInput").ap()

    xT_own_d = din("xT_own", [D, T])
    xT_oth_d = din("xT_oth", [D, T])
    xT_halo_d = din("xT_halo", [D, 64])
    x_own_d = din("x_own", [T, D])
    pos_d = din("pos", [64, 2 * T], I32)
    cst_d = din("cst", [128, C_END])
    sel_d = din("selc", [32, NE * 128])
    cw_d = din("cw", [128, 8, 3])
    gq_d = din("gq", [128, 3])
    gkv_d = din("gkv", [128, 2])
    lnp_d = din("lnp", [4, 128, D])
    wr_d = din("w_r", [128, 8, NE])
    br_d = din("b_r", [128, NE])
    bgu_d = din("b_gu", [128, NE, 16])
    bdn_d = din("b_dn", [NE, D])
    w_inA_d = din("w_inA", [D, 3072])
    w_inQ_d = din("w_inQ", [D, 384])
    w_inK_d = din("w_inK", [D, 384])
    w_mrg_d = din("w_mrg", [D, 4096])
    w_uq_d = din("w_uq", [384, 2048])
    w_uk_d = din("w_uk", [256, 1024])
    w_uv_d = din("w_uv", [256, 1024])
    w_out_d = din("w_out", [D, D])
    w_gu_d = din("w_gu", [NE, D, 2 * D])
    w_dn_d = din("w_dn", [NE, D, D])
    out_d = nc.dram_tensor("out", [T, D], F32, kind="ExternalOutput").ap()
    xg_d = nc.dram_tensor("xg_scr", [NSLOT, D], BF16, kind="Internal").ap()
    eo_d = nc.dram_tensor("eo_scr", [NSLOT + 128, D], F32, kind="Internal").ap()
    x1_d = nc.dram_tensor("x1_scr", [T, D], F32, kind="Internal").ap()

    def kp(ap):
        return ap.rearrange("(k p) n -> p k n", p=128)

    with ExitStack() as st0x:
        st0 = st0x
        S = Sched(nc, st0x)

        dsts2 = st0.enter_context(nc.sbuf_tensor("dsts", [128, 64], I32))
        dstg2 = st0.enter_context(nc.sbuf_tensor("dstg", [128, 64], I32))
        reg_sc = nc.gpsimd.alloc_register("bc_scatter")
        nc.gpsimd.reg_mov(reg_sc, NSLOT - 1)
        reg_ga = nc.gpsimd.alloc_register("bc_gather")
        nc.gpsimd.reg_mov(reg_ga, NSLOT + 127)
        ARENA_BYTES = 207 * 1024
        arena = st0.enter_context(nc.sbuf_tensor("arena", [128, ARENA_BYTES // 2], BF16))
        DTSZ = {F32: 4, BF16: 2, I32: 4, U32: 4}

        class Region:
            def __init__(self, base_kb, limit_kb):
                self.ptr, self.limit = int(base_kb * 1024), int(limit_kb * 1024)

            def alloc(self, shape, dt):
                n = int(np.prod(shape[1:])) * DTSZ[dt]
                n = (n + 63) // 64 * 64
                off = self.ptr
                self.ptr += n
                if self.ptr > self.limit:
                    raise RuntimeError(f"region overflow {self.ptr} > {self.limit}")
                nel = int(np.prod(shape[1:]))
                v = arena[0:shape[0], off // 2: off // 2 + nel * DTSZ[dt] // 2]
                if dt != BF16:
                    v = v.bitcast(dt)
                if len(shape) == 3:
                    v = v.rearrange("p (a b) -> p a b", a=shape[1])
                return v

        def sb(reg, name, shape, dt):
            return reg.alloc(list(shape), dt)

        st0 = Region(0, 6)
        P1 = Region(6, 70)
        KQ = Region(70, 122)
        RX = Region(122, 154)
        W1a = Region(154, 166)
        TKQ = Region(166, 207)
        TA = Region(166, 207)
        st1b = Region(122, 207)
        st1c = Region(70, 207)
        st2 = Region(102, 207)
        st3 = Region(6, 207)

        def MM(out, lhsT, rhs, st, sp, R, W, sig=None):
            S.op("pe", lambda t: t.matmul(out, lhsT=lhsT, rhs=rhs, start=st, stop=sp), R, W,
                 signal=(sp if sig is None else sig))

        def ACT(out, in_, func, R, W, **kw):
            S.op("act", lambda a: a.activation(out=out, in_=in_, func=func, **kw), R, W)

        def TT(e, out, a, b, op, R, W):
            S.op(e, lambda v: v.tensor_tensor(out=out, in0=a, in1=b, op=op), R, W)

        def TS(e, out, a, s1, s2, op0, op1, R, W):
            S.op(e, lambda v: v.tensor_scalar(out=out, in0=a, scalar1=s1, scalar2=s2, op0=op0, op1=op1), R, W)

        def STT(out, a, s, b, op0, op1, R, W, **kw):
            S.op("dve", lambda v: v.scalar_tensor_tensor(out=out, in0=a, scalar=s, in1=b, op0=op0, op1=op1, **kw), R, W)

        def CP(e, out, in_, R, W):
            if e == "act":
                S.op(e, lambda a: a.activation(out=out, in_=in_, func=AF.Copy), R, W)
            else:
                S.op(e, lambda v: v.tensor_copy(out=out, in_=in_), R, W)

        def LD(q, out, in_, W, R=()):
            return S.dma(q, lambda g: g.dma_start(out=out, in_=in_), reads=R, writes=W)

        PS = st0x.enter_context(nc.psum_tensor("PS", [128, 8, 512], F32))
        BP = [Buf(f"ps{i}") for i in range(8)]

        cst = sb(st0, "cst", [128, C_END], F32)
        B_c = Buf("cst")
        LD("sp", cst[:], cst_d, [B_c])
        cbf = sb(st0, "cbf", [128, C_IOTA], BF16)
        B_cb = Buf("cbf")
        CP("dve", cbf[:], cst[:, 0:C_IOTA], [B_c], [B_cb])
        ident_f = cst[:, C_ID:C_ID + 128]
        ones_f = cst[:, C_ONE:C_ONE + 128]
        ident_b = cbf[:, C_ID:C_ID + 128]
        ones_b = cbf[:, C_ONE:C_ONE + 128]
        mask_b = [cbf[:, C_MOWN:C_MOWN + 128], cbf[:, C_MOTH:C_MOTH + 128]]
        tri_b = cbf[:, C_TRI:C_TRI + 128]
        iota_f = cst[:, C_IOTA:C_IOTA + 32]
        invf = cst[0:64, C_INVF:C_INVF + 1]
        sgn = cst[0:64, C_SGN:C_SGN + 1]
        dsts = dsts2[:].rearrange("p (a b) -> p a b", b=4)
        dstg = dstg2[:].rearrange("p (a b) -> p a b", b=4)
        wk = sb(st0, "wk", [128, 16, 4], F32)

        xTo = sb(RX, "xTo", [128, 8, T], BF16)
        B_xTo = [Buf(f"xTo{s}") for s in range(NSEG)]
        ckvT = sb(KQ, "ckvT", [128, 2, 2 * T], BF16)
        kpeT = sb(KQ, "kpeT", [64, 2 * T], BF16)
        B_kv = [Buf(f"kv{s}") for s in range(8)]
        qlT = sb(KQ, "qlT", [128, 3, T], BF16)
        B_ql = [Buf(f"ql{s}") for s in range(NSEG)]
        ycT = sb(P1, "ycT", [128, 8, T], BF16)
        B_yc = [Buf(f"yc{f}") for f in range(8)]
        cso = sb(KQ, "cso", [64, T], F32)
        sno = sb(KQ, "sno", [64, T], F32)
        B_cs = [Buf(f"cs{s}") for s in range(NSEG)]
        attnT = sb(P1, "attnT", [128, 8, T], BF16)
        B_at = [[Buf(f"at{h}_{j}") for j in range(NSEG)] for h in range(8)]

        st1a = TKQ
        wsl = [sb(W1a, f"wsl{i}", [128, 8, 384], BF16) for i in range(2)]
        B_wsl = [Buf("wsl0"), Buf("wsl1")]
        xob0 = sb(st1a, "xob0", [128, 8, SEG], BF16)
        xob = [xob0, xob0]
        B_xob0 = Buf("xob0")
        B_xob = [B_xob0, B_xob0]
        posi = sb(st1a, "posi", [64, SEG], I32)
        B_pos = Buf("pos")
        gq = sb(st1a, "gq", [128, 3], F32)
        gkv = sb(st1a, "gkv", [128, 2], F32)
        B_gn = Buf("gains")
        LD("sp", gq[:], gq_d, [B_gn])
        LD("sp", gkv[:], gkv_d, [B_gn])
        raw = sb(st1a, "raw", [128, 3, SEG], F32)
        sq = sb(st1a, "sq", [128, 3, SEG], F32)
        B_raw, B_sq = Buf("raw"), Buf("sq")
        rk = sb(st1a, "rk", [128, SEG], F32)
        B_rk = Buf("rk")
        tA = sb(st1a, "tA", [64, SEG], F32)
        tB = sb(st1a, "tB", [64, SEG], F32)
        B_tA, B_tB = Buf("tA"), Buf("tB")
        cst_t = sb(st1a, "cst_t", [64, SEG], F32)
        snt_t = sb(st1a, "snt_t", [64, SEG], F32)
        B_cst = Buf("cst_t")
        rp1 = sb(st1a, "rp1", [64, SEG], F32)
        rp2 = sb(st1a, "rp2", [64, SEG], F32)
        rp3 = sb(st1a, "rp3", [64, SEG], F32)
        B_rp = Buf("rp")

        def rope_tables(col0, cs_out, sn_out, Bout):
            LD("sp", posi[:], pos_d[:, col0:col0 + SEG], [B_pos])
            CP("dve", rp1[:], posi[:], [B_pos], [B_rp])
            TS("dve", rp1[:], rp1[:], invf, None, ALU.mult, ALU.bypass, [B_c, B_rp], [B_rp])
            TS("dve", rp2[:], rp1[:], MAGIC, MAGIC, ALU.add, ALU.subtract, [B_rp], [B_rp])
            TT("dve", rp2[:], rp1[:], rp2[:], ALU.subtract, [B_rp], [B_rp])
            ACT(sn_out, rp2[:], AF.Sin, [B_rp], [Bout], scale=TWO_PI_S)
            TS("dve", sn_out, sn_out, sgn, None, ALU.mult, ALU.bypass, [B_c, Bout], [Bout])
            TS("dve", rp1[:], rp1[:], 0.25, None, ALU.add, ALU.bypass, [B_rp], [B_rp])
            TS("dve", rp3[:], rp1[:], MAGIC, MAGIC, ALU.add, ALU.subtract, [B_rp], [B_rp])
            TT("dve", rp3[:], rp1[:], rp3[:], ALU.subtract, [B_rp], [B_rp])
            ACT(cs_out, rp3[:], AF.Sin, [B_rp], [Bout], scale=TWO_PI_S)

        LD("pool", wsl[0][:], kp(w_inK_d), [B_wsl[0]])
        for s in range(NSEG):
            LD("pool", xTo[:, :, s * SEG:(s + 1) * SEG], kp(xT_own_d[:, s * SEG:(s + 1) * SEG]), [B_xTo[s]])
            if s == 0:
                LD("pool", wsl[1][:], kp(w_inQ_d), [B_wsl[1]])
        korder = [0, 4, 1, 5, 2, 6, 3, 7]

        def kseg(s):
            if s < NSEG:
                return (xTo[:, :, s * SEG:(s + 1) * SEG], B_xTo[s], cso[:, s * SEG:(s + 1) * SEG], sno[:, s * SEG:(s + 1) * SEG], B_cs[s])
            return (xob[0][:], B_xob[0], cst_t[:], snt_t[:], B_cst)

        rope_tables(korder[0] * SEG, *kseg(korder[0])[2:])
        for it, s in enumerate(korder):
            xs, Bx, cs_ap, sn_ap, Bt = kseg(s)
            if s >= NSEG:
                LD("pool", xob[0][:], kp(xT_oth_d[:, (s - 4) * SEG:(s - 3) * SEG]), [Bx])
            o = 4 * (it % 2)
            for c in range(2):
                for kc in range(8):
                    MM(PS[:, o + c, :], wsl[0][:, kc, c * 128:(c + 1) * 128], xs[:, kc, :], kc == 0, kc == 7, [B_wsl[0], Bx], [BP[o + c]])
            for c in range(2):
                for kc in range(8):
                    MM(PS[0:64, o + 2 + c, :], wsl[0][:, kc, 256 + c * 64:320 + c * 64], xs[:, kc, :], kc == 0, kc == 7, [B_wsl[0], Bx], [BP[o + 2 + c]])
            if it + 1 < 8:
                s2 = korder[it + 1]
                rope_tables(s2 * SEG, *kseg(s2)[2:])
            for c in range(2):
                ACT(raw[:, c, :], PS[:, o + c, :], AF.Copy, [BP[o + c]], [B_raw])
                ACT(sq[:, c, :], PS[:, o + c, :], AF.Square, [BP[o + c]], [B_sq])
            for c in range(2):
                MM(PS[:, o, :], ones_f, sq[:, c, :], c == 0, c == 1, [B_c, B_sq], [BP[o]])
            ACT(rk[:], PS[:, o, :], AF.Ln, [BP[o]], [B_rk], scale=1.0 / 256.0, bias=RMS_EPS)
            ACT(rk[:], rk[:], AF.Exp, [B_rk], [B_rk], scale=-0.5)
            for c in range(2):
                STT(ckvT[:, c, s * SEG:(s + 1) * SEG], raw[:, c, :], gkv[:, c:c + 1], rk[:], ALU.mult, ALU.mult, [B_raw, B_rk, B_gn], [B_kv[s]])
            TT("dve", tA[:], PS[0:64, o + 2, :], cs_ap, ALU.mult, [BP[o + 2], Bt], [B_tA])
            TT("dve", tB[:], PS[0:64, o + 3, :], sn_ap, ALU.mult, [BP[o + 3], Bt], [B_tB])
            TT("dve", kpeT[:, s * SEG:(s + 1) * SEG], tA[:], tB[:], ALU.add, [B_tA, B_tB], [B_kv[s]])

        for s in range(NSEG):
            xs, Bx = xTo[:, :, s * SEG:(s + 1) * SEG], B_xTo[s]
            o = 4 * (s % 2)
            for c in range(3):
                for kc in range(8):
                    MM(PS[:, o + c, :], wsl[1][:, kc, c * 128:(c + 1) * 128], xs[:, kc, :], kc == 0, kc == 7, [B_wsl[1], Bx], [BP[o + c]])
            for c in range(3):
                ACT(raw[:, c, :], PS[:, o + c, :], AF.Copy, [BP[o + c]], [B_raw])
                ACT(sq[:, c, :], PS[:, o + c, :], AF.Square, [BP[o + c]], [B_sq])
            for c in range(3):
                MM(PS[:, o + 3, :], ones_f, sq[:, c, :], c == 0, c == 2, [B_c, B_sq], [BP[o + 3]])
            ACT(rk[:], PS[:, o + 3, :], AF.Ln, [BP[o + 3]], [B_rk], scale=1.0 / 384.0, bias=RMS_EPS)
            ACT(rk[:], rk[:], AF.Exp, [B_rk], [B_rk], scale=-0.5)
            for c in range(3):
                STT(qlT[:, c, s * SEG:(s + 1) * SEG], raw[:, c, :], gq[:, c:c + 1], rk[:], ALU.mult, ALU.mult, [B_raw, B_rk, B_gn], [B_ql[s]])

        S.barrier()
        st1a = TA
        cw = sb(st1a, "cw", [128, 8, 3], F32)
        B_par = Buf("par1a")
        LD("sp", cw[:], cw_d, [B_par])
        xh = sb(st1a, "xh", [128, 8, 64], BF16)
        B_xh = Buf("xh")
        LD("pool", xh[:], kp(xT_halo_d), [B_xh])
        uext = sb(st1a, "uext", [128, 32, 66], F32)
        B_u = Buf("uext")
        gbs = sb(st1a, "gbs", [128, T], F32)
        B_gbs = Buf("gbs")
        gcs = [sb(st1a, f"gcs{i}", [128, SEG], F32) for i in range(2)]
        B_gcs = [Buf("gcs0"), Buf("gcs1")]
        yv = sb(st1a, "yv", [128, 32, 64], F32)
        B_yv = Buf("yv")
        for f in range(8):
            w_, Bw = wsl[f % 2], B_wsl[f % 2]
            LD("pool", w_[:], kp(w_inA_d[:, f * 384:(f + 1) * 384]), [Bw])
            for c in range(2):
                for kc in range(8):
                    MM(PS[:, 6 + c, 0:64], w_[:, kc, 128 + c * 128:256 + c * 128], xh[:, kc, :], kc == 0, kc == 7, [Bw, B_xh], [BP[6 + c]])
            CP("act", gcs[0][:, 0:64], PS[:, 6, 0:64], [BP[6]], [B_gcs[0]])
            TT("dve", uext[:, :, 0:2], gcs[0][:, 0:64].rearrange("p (c r) -> p c r", r=2), PS[:, 7, 0:64].rearrange("p (c r) -> p c r", r=2),
               ALU.mult, [B_gcs[0], BP[7]], [B_u])
            for s in range(NSEG):
                xs, Bx = xTo[:, :, s * SEG:(s + 1) * SEG], B_xTo[s]
                for c in range(3):
                    bk = 3 * (s % 2) + c
                    for kc in range(8):
                        MM(PS[:, bk, :], w_[:, kc, c * 128:(c + 1) * 128], xs[:, kc, :], kc == 0, kc == 7, [Bw, Bx], [BP[bk]])
                b0 = 3 * (s % 2)
                CP("act", gbs[:, s * SEG:(s + 1) * SEG], PS[:, b0, :], [BP[b0]], [B_gbs])
                CP("act", gcs[s % 2][:], PS[:, b0 + 1, :], [BP[b0 + 1]], [B_gcs[s % 2]])
                TT("dve", uext[:, 8 * s:8 * s + 8, 2:66], gcs[s % 2][:].rearrange("p (c r) -> p c r", r=64),
                   PS[:, b0 + 2, :].rearrange("p (c r) -> p c r", r=64), ALU.mult, [B_gcs[s % 2], BP[b0 + 2]], [B_u])
            TS("dve", yv[:], uext[:, :, 2:66], cw[:, f, 2:3], None, ALU.mult, ALU.bypass, [B_u, B_par], [B_yv])
            STT(yv[:], uext[:, :, 1:65], cw[:, f, 1:2], yv[:], ALU.mult, ALU.add, [B_u, B_par, B_yv], [B_yv])
            STT(yv[:], uext[:, :, 0:64], cw[:, f, 0:1], yv[:], ALU.mult, ALU.add, [B_u, B_par, B_yv], [B_yv])
            TT("dve", ycT[:, f, :], gbs[:], yv[:].rearrange("p c r -> p (c r)"), ALU.mult, [B_gbs, B_yv], [B_yc[f]])
        S.barrier()

        wuq = sb(st1b, "wuq", [128, 3, 2048], BF16)
        wuk = sb(st1b, "wuk", [128, 2, 1024], BF16)
        wuv = sb(st1b, "wuv", [128, 2, 1024], BF16)
        B_wu = Buf("wu")
        LD("pool", wuq[:], kp(w_uq_d), [B_wu])
        LD("pool", wuk[:], kp(w_uk_d), [B_wu])
        LD("pool", wuv[:], kp(w_uv_d), [B_wu])
        knT = [sb(st1b, f"knT{i}", [128, 2 * T], BF16) for i in range(2)]
        Vh = [sb(st1b, f"Vh{i}", [128, 32, 128], BF16) for i in range(2)]
        qn = [sb(st1b, f"qn{i}", [128, T], BF16) for i in range(2)]
        qr = [sb(st1b, f"qr{i}", [64, T], BF16) for i in range(2)]
        B_kn = [[Buf(f"kn{i}_{s}") for s in range(8)] for i in range(2)]
        B_V = [[Buf(f"V{i}_{g}") for g in range(8)] for i in range(2)]
        B_qn = [[Buf(f"qn{i}_{s}") for s in range(NSEG)] for i in range(2)]
        B_qr = [[Buf(f"qr{i}_{s}") for s in range(NSEG)] for i in range(2)]
        qa = sb(st1b, "qa", [64, SEG], F32)
        qb = sb(st1b, "qb", [64, SEG], F32)
        B_qa, B_qb = Buf("qa"), Buf("qb")
        NPT = 4
        pT = [sb(st1b, f"pT{i}", [128, SEG], BF16) for i in range(NPT)]
        B_pT = [Buf(f"pT{i}") for i in range(NPT)]
        rL = [sb(st1b, f"rL{i}", [128, SEG], F32) for i in range(2)]
        B_rL = [Buf("rL0"), Buf("rL1")]
        accL = [sb(st1b, f"accL{i}", [128, SEG], F32) for i in range(2)]
        B_acc = [Buf("accL0"), Buf("accL1")]
        B_xg = Buf("xg_d")
        zsrc = attnT[:, 4:8, :]
        B_zs = [B_at[hh][jj] for hh in range(4, 8) for jj in range(NSEG)]
        S.op("pool", lambda g: g.memset(zsrc, 0.0), [], B_zs)
        xgz = xg_d.rearrange("(p a) n -> p a n", p=128)
        zview = zsrc.rearrange("p a (b c) -> p (a b) c", c=D)
        for i in range(NSLOT // 128 // 8):
            S.dma("sp", lambda g: g.dma_start(out=xgz[:, 8 * i:8 * i + 8, :], in_=zview), B_zs, [B_xg])

        def prep_units(h):
            i = h % 2
            units = []

            def k_unit(s):
                def f():
                    bk = 6 + s % 2
                    for kc in range(2):
                        MM(PS[:, bk, :], wuk[:, kc, h * 128:(h + 1) * 128], ckvT[:, kc, s * SEG:(s + 1) * SEG], kc == 0, kc == 1, [B_wu, B_kv[s]], [BP[bk]])
                    CP("act", knT[i][:, s * SEG:(s + 1) * SEG], PS[:, bk, :], [BP[bk]], [B_kn[i][s]])
                return f

            def v_unit(g):
                def f():
                    bk = 6 + g % 2
                    for ii in range(4):
                        tt = 4 * g + ii
                        for kc in range(2):
                            MM(PS[:, bk, ii * 128:(ii + 1) * 128], ckvT[:, kc, tt * 128:(tt + 1) * 128], wuv[:, kc, h * 128:(h + 1) * 128],
                               kc == 0, kc == 1, [B_wu, B_kv[tt // 4]], [BP[bk]], sig=(kc == 1 and ii == 3))
                    CP("act", Vh[i][:, 4 * g:4 * g + 4, :].rearrange("p a b -> p (a b)"), PS[:, bk, :], [BP[bk]], [B_V[i][g]])
                return f

            def q_unit(s):
                def f():
                    cols = slice(s * SEG, (s + 1) * SEG)
                    for kc in range(3):
                        MM(PS[:, 6, :], wuq[:, kc, h * 128:(h + 1) * 128], qlT[:, kc, cols], kc == 0, kc == 2, [B_wu, B_ql[s]], [BP[6]])
                    for kc in range(3):
                        MM(PS[0:64, 7, :], wuq[:, kc, 1024 + h * 64:1088 + h * 64], qlT[:, kc, cols], kc == 0, kc == 2, [B_wu, B_ql[s]], [BP[7]])
                    CP("act", qn[i][:, cols], PS[:, 6, :], [BP[6]], [B_qn[i][s]])
                    TT("dve", qa[:], PS[0:64, 7, :], cso[:, cols], ALU.mult, [BP[7], B_cs[s]], [B_qa])
                    for kc in range(3):
                        MM(PS[0:64, 6, :], wuq[:, kc, 1536 + h * 64:1600 + h * 64], qlT[:, kc, cols], kc == 0, kc == 2, [B_wu, B_ql[s]], [BP[6]])
                    TT("dve", qb[:], PS[0:64, 6, :], sno[:, cols], ALU.mult, [BP[6], B_cs[s]], [B_qb])
                    TT("pool", qr[i][:, cols], qa[:], qb[:], ALU.add, [B_qa, B_qb], [B_qr[i][s]])
                return f

            for s in range(8):
                units.append(k_unit(s))
                units.append(v_unit(s))
            for s in range(NSEG):
                units.append(q_unit(s))
            order = [16, 0, 1, 8, 9, 17, 2, 3, 10, 11, 18, 4, 5, 12, 13, 19, 6, 7, 14, 15]
            return [units[k] for k in order]

        for u in prep_units(0):
            u()
        seg_ctr = 0
        tile_ctr = 0
        LOOK = 2
        for h in range(8):
            i = h % 2
            nxt = prep_units(h + 1) if h + 1 < 8 else []
            tiles = []
            for j in range(NSEG):
                tl = []
                for m in range(4):
                    tl.append((j * SEG + m * 128, m * 128, 0))
                    tl.append((T + j * SEG + m * 128, m * 128, 1))
                for js in range(j):
                    for m in range(4):
                        tl.append((js * SEG + m * 128, 0, None))
                        tl.append((T + js * SEG + m * 128, 0, None))
                tl.sort(key=lambda t: t[1])
                for ti, (k0, q0, mi) in enumerate(tl):
                    tiles.append((j, k0, q0, mi, ti == 0, ti == len(tl) - 1))
            nt = len(tiles)
            segbuf = {}
            for idx in range(nt + LOOK):
                if idx < nt:
                    j, k0, q0, mi, first, last = tiles[idx]
                    if first:
                        segbuf[j] = seg_ctr
                        seg_ctr += 1
                    bs = (tile_ctr + idx) % 3
                    pb = (tile_ctr + idx) % NPT
                    nq = SEG - q0
                    qc0 = j * SEG + q0
                    ks = k0 // SEG
                    MM(PS[:, bs, 0:nq], knT[i][:, k0:k0 + 128], qn[i][:, qc0:qc0 + nq], True, False, [B_kn[i][ks], B_qn[i][j]], [BP[bs]])
                    MM(PS[:, bs, 0:nq], kpeT[:, k0:k0 + 128], qr[i][:, qc0:qc0 + nq], False, True, [B_kv[ks], B_qr[i][j]], [BP[bs]])
                    ACT(pT[pb][:, 0:nq], PS[:, bs, 0:nq], AF.Exp, [BP[bs]], [B_pT[pb]], scale=QK_SCALE)
                    if mi is not None:
                        TT("dve", pT[pb][:, 0:128], pT[pb][:, 0:128], mask_b[mi], ALU.mult, [B_pT[pb], B_cb], [B_pT[pb]])
                    sc = segbuf[j] % 2
                    if first:
                        CP("dve", accL[sc][:], pT[pb][:], [B_pT[pb]], [B_acc[sc]])
                    else:
                        TT("dve", accL[sc][:, q0:SEG], accL[sc][:, q0:SEG], pT[pb][:, 0:nq], ALU.add, [B_acc[sc], B_pT[pb]], [B_acc[sc]])
                if idx >= LOOK:
                    j, k0, q0, mi, first, last = tiles[idx - LOOK]
                    pb = (tile_ctr + idx - LOOK) % NPT
                    nq = SEG - q0
                    sc = segbuf[j] % 2
                    bo = 3 + sc
                    MM(PS[:, bo, q0:SEG], Vh[i][:, k0 // 128, :], pT[pb][:, 0:nq], first, last, [B_V[i][k0 // SEG], B_pT[pb]], [BP[bo]], sig=True)
                    if last:
                        MM(PS[:, 5, :], ones_f, accL[sc][:], True, True, [B_c, B_acc[sc]], [BP[5]])
                        S.op("dve", lambda v: v.reciprocal(out=rL[sc][:], in_=PS[:, 5, :]), [BP[5]], [B_rL[sc]])
                        TT("dve", attnT[:, h, j * SEG:(j + 1) * SEG], PS[:, bo, :], rL[sc][:], ALU.mult, [BP[bo], B_rL[sc]], [B_at[h][j]])
                if nxt and idx % 4 == 3:
                    nxt.pop(0)()
            for u in nxt:
                u()
            tile_ctr += nt
        S.barrier()

        xTo = sb(st1c, "xTo2", [128, 8, T], BF16)
        B_xTo = [Buf(f"xTo2{s}") for s in range(NSEG)]
        mgT = sb(st1c, "mgT", [128, 8, T], BF16)
        B_mg = [Buf(f"mg{s}") for s in range(NSEG)]
        mark_wm = st1c.ptr
        wm = [sb(st1c, f"wm{i}", [128, 8, 512], BF16) for i in range(2)]
        B_wm = [Buf("wm0"), Buf("wm1")]
        wo = sb(st1c, "wo", [128, 8, D], BF16)
        B_wo = Buf("wo")
        LD("pool", wm[0][:], kp(w_mrg_d[:, 0:512]), [B_wm[0]])
        for s in range(NSEG):
            LD("pool", xTo[:, :, s * SEG:(s + 1) * SEG], kp(xT_own_d[:, s * SEG:(s + 1) * SEG]), [B_xTo[s]])
        LD("pool", wm[1][:], kp(w_mrg_d[:, 512:1024]), [B_wm[1]])
        LD("pool", wo[:], kp(w_out_d), [B_wo])
        mark_1c = st1c.ptr
        sgc = [sb(st1c, f"sgc{i}", [128, SEG], F32) for i in range(2)]
        sga = [sb(st1c, f"sga{i}", [128, SEG], F32) for i in range(2)]
        B_sgc = [Buf("sgc0"), Buf("sgc1")]
        B_sga = [Buf("sga0"), Buf("sga1")]
        all_at = [B_at[h][j] for h in range(8) for j in range(NSEG)]
        it = 0
        for f in range(8):
            w_, Bw = wm[f % 2], B_wm[f % 2]
            if 1 <= f < 7:
                LD("pool", wm[(f + 1) % 2][:], kp(w_mrg_d[:, (f + 1) * 512:(f + 2) * 512]), [B_wm[(f + 1) % 2]])
            for s in range(NSEG):
                b0 = 4 * (it % 2)
                i2 = it % 2
                it += 1
                cols = slice(s * SEG, (s + 1) * SEG)
                for kc in range(8):
                    MM(PS[:, b0, :], w_[:, kc, 0:128], ycT[:, kc, cols], kc == 0, kc == 7, [Bw, B_yc[kc]], [BP[b0]])
                for kc in range(8):
                    MM(PS[:, b0 + 1, :], w_[:, kc, 128:256], attnT[:, kc, cols], kc == 0, kc == 7, [Bw, B_at[kc][s]], [BP[b0 + 1]])
                for kc in range(8):
                    MM(PS[:, b0 + 2, :], w_[:, kc, 256:384], xTo[:, kc, cols], kc == 0, kc == 7, [Bw, B_xTo[s]], [BP[b0 + 2]])
                for kc in range(8):
                    MM(PS[:, b0 + 3, :], w_[:, kc, 384:512], xTo[:, kc, cols], kc == 0, kc == 7, [Bw, B_xTo[s]], [BP[b0 + 3]])
                ACT(sgc[i2][:], PS[:, b0 + 2, :], AF.Sigmoid, [BP[b0 + 2]], [B_sgc[i2]])
                ACT(sga[i2][:], PS[:, b0 + 3, :], AF.Sigmoid, [BP[b0 + 3]], [B_sga[i2]])
                TT("dve", sgc[i2][:], sgc[i2][:], PS[:, b0, :], ALU.mult, [B_sgc[i2], BP[b0]], [B_sgc[i2]])
                TT("dve", sga[i2][:], sga[i2][:], PS[:, b0 + 1, :], ALU.mult, [B_sga[i2], BP[b0 + 1]], [B_sga[i2]])
                TT("pool", mgT[:, f, cols], sgc[i2][:], sga[i2][:], ALU.add, [B_sgc[i2], B_sga[i2]], [B_mg[s]])

        lng = sb(st1c, "lng", [128, D], F32)
        lnb = sb(st1c, "lnb", [128, D], F32)
        B_ln = Buf("ln1")
        LD("sp", lng[:], lnp_d[0], [B_ln])
        LD("sp", lnb[:], lnp_d[1], [B_ln])
        wrt = sb(st1c, "wrt", [128, 8, NE], F32)
        brt = sb(st1c, "brt", [128, NE], F32)
        B_wr = Buf("wr")
        LD("sp", wrt[:], wr_d, [B_wr])
        LD("sp", brt[:], br_d, [B_wr])
        S.barrier()
        mark_2 = st1c.ptr
        st1c.ptr = mark_1c
        xt = [sb(st1c, f"xt{i}", [128, D], F32) for i in range(2)]
        B_xt = [Buf("xt0"), Buf("xt1")]
        st1c.ptr = max(st1c.ptr, mark_2)
        NH, LA = 5, 4
        h1 = [sb(st1c, f"h1{i}", [128, D], F32) for i in range(2)]
        Rwm = Region(mark_wm / 1024.0, mark_wm / 1024.0 + 16)
        h1 += [sb(Rwm, f"h1{i}", [128, D], F32) for i in range(2, NH)]
        B_h1 = [Buf(f"h1{i}") for i in range(NH)]
        xlo = [sb(Rwm, f"xlo{i}", [128, D], BF16) for i in range(2)]
        B_xlo = [Buf("xlo0"), Buf("xlo1")]
        x1b_all = xTo.rearrange("p a (b c) -> p (a b) c", c=D)
        B_x1b = [Buf(f"x1b{t}") for t in range(16)]
        x1Th = sb(st1c, "x1Th", [128, 8, 128], BF16)
        x1Tl = sb(st1c, "x1Tl", [128, 8, 128], BF16)
        B_x1T = Buf("x1T")
        wrh = sb(st1c, "wrh", [128, 8, NE], BF16)
        wrl = sb(st1c, "wrl", [128, 8, NE], BF16)
        wrd = sb(st1c, "wrd", [128, 8, NE], F32)
        B_wrs = Buf("wr_split")
        CP("dve", wrh[:], wrt[:], [B_wr], [B_wrs])
        TT("dve", wrd[:], wrt[:], wrh[:], ALU.subtract, [B_wr, B_wrs], [B_wrs])
        CP("dve", wrl[:], wrd[:], [B_wrs], [B_wrs])
        st6 = [sb(st1c, f"st6{i}", [128, 2, 6], F32) for i in range(2)]
        mv = [sb(st1c, f"mv{i}", [128, 2], F32) for i in range(2)]
        rs = [sb(st1c, f"rs{i}", [128, 1], F32) for i in range(2)]
        nb = [sb(st1c, f"nb{i}", [128, 1], F32) for i in range(2)]
        B_st = [Buf("st0"), Buf("st1")]
        lgt_all = sb(st1c, "lgt_all", [128, 16, NE], F32)
        top8_all = sb(st1c, "top8_all", [128, 16, 8], F32)
        idx8_all = sb(st1c, "idx8_all", [128, 16, 8], U32)
        B_lg = [Buf(f"lg{t}") for t in range(16)]
        B_route = [Buf(f"route{t}") for t in range(16)]
        B_x1d = [Buf(f"x1d{t}") for t in range(16)]

        def stageA(tt):
            s = tt // 4
            i2 = tt % 2
            i3 = tt % NH
            tcols = slice(tt * 128, (tt + 1) * 128)
            LD("sp", xt[i2][:], x_own_d[tt * 128:(tt + 1) * 128, :], [B_xt[i2]])
            for hf in range(2):
                for kc in range(8):
                    MM(PS[:, 2 * i2 + hf, :], mgT[:, kc, tcols], wo[:, kc, hf * 512:(hf + 1) * 512], kc == 0, kc == 7, [B_mg[s], B_wo], [BP[2 * i2 + hf]])
            psv = PS[:, 2 * i2:2 * i2 + 2, :].rearrange("p a b -> p (a b)")
            STT(h1[i3][:], xt[i2][:], ALPHA, psv, ALU.mult, ALU.add, [B_xt[i2], BP[2 * i2], BP[2 * i2 + 1]], [B_h1[i3]])
            for hf in range(2):
                S.op("dve", lambda v: v.bn_stats(out=st6[i2][:, hf, :], in_=h1[i3][:, hf * 512:(hf + 1) * 512]), [B_h1[i3]], [B_st[i2]])
            S.op("dve", lambda v: v.bn_aggr(out=mv[i2][:], in_=st6[i2][:].rearrange("p a b -> p (a b)")), [B_st[i2]], [B_st[i2]])
            ACT(rs[i2][:], mv[i2][:, 1:2], AF.Sqrt, [B_st[i2]], [B_st[i2]], scale=1.0, bias=LN_EPS)
            S.op("dve", lambda v: v.reciprocal(out=rs[i2][:], in_=rs[i2][:]), [B_st[i2]], [B_st[i2]])
            STT(nb[i2][:], mv[i2][:, 0:1], -1.0, rs[i2][:], ALU.mult, ALU.mult, [B_st[i2]], [B_st[i2]])
            ACT(h1[i3][:], h1[i3][:], AF.Identity, [B_h1[i3], B_st[i2]], [B_h1[i3]], scale=rs[i2][:, 0:1], bias=nb[i2][:, 0:1])
            TT("dve", h1[i3][:], h1[i3][:], lng[:], ALU.mult, [B_h1[i3], B_ln], [B_h1[i3]])
            TT("pool", h1[i3][:], h1[i3][:], lnb[:], ALU.add, [B_h1[i3], B_ln], [B_h1[i3]])
            if stage == 1:
                S.dma("sp", lambda g: g.dma_start(out=out_d[tt * 128:(tt + 1) * 128, :], in_=h1[i3][:]), [B_h1[i3]], [], is_output=True)
                return
            S.dma("sp", lambda g: g.dma_start(out=x1_d[tt * 128:(tt + 1) * 128, :], in_=h1[i3][:]), [B_h1[i3]], [B_x1d[tt]])
            CP("act", x1b_all[:, tt, :], h1[i3][:], [B_h1[i3]], [B_x1b[tt]])

        def stageB(tt):
            i2 = tt % 2
            i3 = tt % NH
            TT("dve", xlo[i2][:], h1[i3][:], x1b_all[:, tt, :], ALU.subtract, [B_h1[i3], B_x1b[tt]], [B_xlo[i2]])
            PSb4 = PS[:, 4, :].bitcast(BF16)
            PSb5 = PS[:, 5, :].bitcast(BF16)
            for kc in range(8):
                S.op("pe", lambda t: t.transpose(PSb4[:, kc * 128:(kc + 1) * 128], x1b_all[:, tt, kc * 128:(kc + 1) * 128], ident_b),
                     [B_x1b[tt], B_cb], [BP[4]], signal=(kc == 7))
            for kc in range(8):
                S.op("pe", lambda t: t.transpose(PSb5[:, kc * 128:(kc + 1) * 128], xlo[i2][:, kc * 128:(kc + 1) * 128], ident_b),
                     [B_xlo[i2], B_cb], [BP[5]], signal=(kc == 7))
            CP("act", x1Th[:].rearrange("p a b -> p (a b)"), PSb4, [BP[4]], [B_x1T])
            CP("act", x1Tl[:].rearrange("p a b -> p (a b)"), PSb5, [BP[5]], [B_x1T])
            bl = 6 + i2
            for kc in range(8):
                MM(PS[:, bl, 0:NE], x1Th[:, kc, :], wrh[:, kc, :], kc == 0, False, [B_x1T, B_wrs], [BP[bl]])
                MM(PS[:, bl, 0:NE], x1Th[:, kc, :], wrl[:, kc, :], False, False, [B_x1T, B_wrs], [BP[bl]])
                MM(PS[:, bl, 0:NE], x1Tl[:, kc, :], wrh[:, kc, :], False, kc == 7, [B_x1T, B_wrs], [BP[bl]])
            TT("dve", lgt_all[:, tt, :], PS[:, bl, 0:NE], brt[:], ALU.add, [BP[bl], B_wr], [B_lg[tt]])
            S.op("dve", lambda v: v.max(out=top8_all[:, tt, :], in_=lgt_all[:, tt, :]), [B_lg[tt]], [B_lg[tt]])
            S.op("dve", lambda v: v.max_index(out=idx8_all[:, tt, :], in_max=top8_all[:, tt, :], in_values=lgt_all[:, tt, :]), [B_lg[tt]], [B_lg[tt]])

        for tt in range(LA):
            stageA(tt)
        for tt in range(16):
            if tt + LA < 16:
                stageA(tt + LA)
            if stage != 1:
                stageB(tt)
        if stage == 1:
            S.barrier()
            S.finish()
            return nc

        S.barrier()
        st1c.ptr = mark_1c
        bgu = sb(st2, "bgu", [128, NE, 16], F32)
        bdn = sb(st2, "bdn", [NE, D], BF16)
        selb = sb(st2, "selb", [NE, NE * 128], BF16)
        B_bias = Buf("bias")
        LD("sp", bgu[:], bgu_d, [B_bias])
        LD("pool", bdn[:], bdn_d, [B_bias])
        for i in range(2):
            LD("pool", selb[:, i * 2048:(i + 1) * 2048], sel_d[:, i * 2048:(i + 1) * 2048], [B_bias])
        zrow = sb(st2, "zrow", [128, D], F32)
        B_z = Buf("z")
        S.op("pool", lambda g: g.memset(zrow[:], 0.0), [], [B_z])
        B_eo = [Buf(f"eo{e}") for e in range(NE + 1)]
        S.dma("sp", lambda g: g.dma_start(out=eo_d[NSLOT:NSLOT + 128, :], in_=zrow[:]), [B_z], [B_eo[NE]])
        ekf = sb(st1c, "ekf", [128, 16, 4], F32)
        tmp4 = sb(st1c, "tmp4", [128, 16, 4], F32)
        den = sb(st1c, "den", [128, 16], F32)
        mskb = sb(st1c, "mskb", [128, 16, NE], BF16)
        posf = sb(st1c, "posf", [128, 16, NE], F32)
        ohk = sb(st1c, "ohk", [128, 16, NE], F32)
        posk = sb(st1c, "posk", [128, 16, 4], F32)
        dstf = sb(st1c, "dstf", [128, 64], F32)
        ovf = sb(st1c, "ovf", [128, 64], F32)
        B_rt = Buf("router")
        B_wk = Buf("wk")
        CP("dve", ekf[:], idx8_all[:, :, 0:4], B_lg, [B_rt])
        TT("dve", tmp4[:], top8_all[:, :, 0:4], top8_all[:, :, 0:1].broadcast_to([128, 16, 4]), ALU.subtract, B_lg, [B_rt])
        ACT(wk[:], tmp4[:], AF.Exp, [B_rt], [B_wk])
        S.op("dve", lambda v: v.reduce_sum(out=den[:], in_=wk[:], axis=AX.X), [B_wk], [B_rt])
        S.op("dve", lambda v: v.reciprocal(out=den[:], in_=den[:]), [B_rt], [B_rt])
        TT("dve", wk[:], wk[:], den[:].unsqueeze(2).broadcast_to([128, 16, 4]), ALU.mult, [B_wk, B_rt], [B_wk])
        TT("dve", mskb[:], lgt_all[:], top8_all[:, :, 3:4].broadcast_to([128, 16, NE]), ALU.is_ge, B_lg, [B_rt])
        MM(PS[:, 0, :], tri_b, mskb[:].rearrange("p t e -> p (t e)"), True, False, [B_cb, B_rt], [BP[0]], sig=False)
        for t_ in range(15):
            n_ = 15 - t_
            MM(PS[:, 0, (t_ + 1) * NE:16 * NE].rearrange("p (t e) -> p t e", e=NE), ones_b, mskb[:, t_:t_ + 1, :].broadcast_to([128, n_, NE]),
               False, t_ == 14, [B_cb, B_rt], [BP[0]], sig=(t_ == 14))
        CP("act", posf[:].rearrange("p t e -> p (t e)"), PS[:, 0, :], [BP[0]], [B_rt])
        iota_b = iota_f.unsqueeze(1).broadcast_to([128, 16, NE])
        for k in range(4):
            TT("dve", ohk[:], iota_b, ekf[:, :, k:k + 1].broadcast_to([128, 16, NE]), ALU.is_equal, [B_c, B_rt], [B_rt])
            TT("dve", ohk[:], ohk[:], posf[:], ALU.mult, [B_rt], [B_rt])
            S.op("dve", lambda v: v.reduce_sum(out=posk[:, :, k], in_=ohk[:], axis=AX.X), [B_rt], [B_rt])
        poskf = posk[:].rearrange("p t k -> p (t k)")
        ekff = ekf[:].rearrange("p t k -> p (t k)")
        TS("dve", ovf[:], poskf, float(CAP), None, ALU.is_ge, ALU.bypass, [B_rt], [B_rt])
        STT(dstf[:], ekff, float(CAP), poskf, ALU.mult, ALU.add, [B_rt], [B_rt])
        STT(dstf[:], ovf[:], float(4 * NSLOT), dstf[:], ALU.mult, ALU.add, [B_rt], [B_rt])
        B_dst = Buf("dst")
        CP("dve", dsts2[:], dstf[:], [B_rt], [B_dst])
        TS("dve", dstf[:], dstf[:], float(NSLOT), None, ALU.min, ALU.bypass, [B_rt], [B_rt])
        CP("dve", dstg2[:], dstf[:], [B_rt], [B_dst])
        for tt in range(16):
            B_route[tt] = B_dst
        B_route_w = B_wk
        R2w = Region(6, 102)
        wgu0 = sb(R2w, "wgu0", [128, 8, 2 * D], BF16)
        wdn0 = sb(R2w, "wdn0", [128, 8, D], BF16)
        wdn1 = sb(R2w, "wdn1", [128, 8, D], BF16)
        wgu1 = sb(R2w, "wgu1", [128, 8, 2 * D], BF16)
        wgu = [wgu0, wgu1]
        wdn = [wdn0, wdn1]
        B_wgu = [Buf("wgu0"), Buf("wgu1")]
        B_wdn = [Buf("wdn0"), Buf("wdn1")]
        LD("pool", wgu[0][:], kp(w_gu_d[0]), [B_wgu[0]])
        LD("pool", wdn[0][:], kp(w_dn_d[0]), [B_wdn[0]])
        B_xgs = [Buf(f"xgs{i}") for i in range(64)]
        for tt in range(16):
            for k in range(4):
                S.dma("pool", lambda g: g.indirect_dma_start(out=xg_d, out_offset=bass.IndirectOffsetOnAxis(ap=dsts2[:, tt * 4 + k:tt * 4 + k + 1], axis=0),
                                                              in_=x1b_all[:, tt, :], in_offset=None, bounds_check=reg_sc, oob_is_err=False),
                      [B_x1b[tt], B_dst, B_xg], [B_xgs[tt * 4 + k]])
        S.barrier()

        xgt = [sb(st2, f"xgt{i}", [128, 3, D], BF16) for i in range(3)]
        B_xgt = [Buf("xgt0"), Buf("xgt1"), Buf("xgt2")]
        xgT = [sb(st2, f"xgT{i}", [128, 8, CAP], BF16) for i in range(2)]
        B_xgT = [Buf("xgT0"), Buf("xgT1")]
        hidT = [sb(st2, f"hidT{i}", [128, 8, CAP], BF16) for i in range(2)]
        B_hid = [[Buf(f"hid{i}_{m}") for m in range(8)] for i in range(2)]
        gt = [sb(st2, f"gt{i}", [128, CAP], F32) for i in range(2)]
        sg = [sb(st2, f"sg{i}", [128, CAP], F32) for i in range(2)]
        ut = [sb(st2, f"ut{i}", [128, CAP], F32) for i in range(2)]
        B_gt = [Buf("gt0"), Buf("gt1")]
        B_sg = [Buf("sg0"), Buf("sg1")]
        B_ut = [Buf("ut0"), Buf("ut1")]
        eot = [sb(st2, f"eot{i}", [128, D], F32) for i in range(2)]
        B_eot = [Buf("eot0"), Buf("eot1")]
        PSb = [PS[:, 6, :].bitcast(BF16), PS[:, 7, :].bitcast(BF16)]

        def load_expert(e):
            i = e % 2
            if e == 0:
                return
            LD("pool", wgu[i][:], kp(w_gu_d[e]), [B_wgu[i]])
            LD("pool", wdn[i][:], kp(w_dn_d[e]), [B_wdn[i]])

        def load_tokens(e):
            LD("sp", xgt[e % 3][:], xg_d[e * CAP:(e + 1) * CAP, :].rearrange("(a p) n -> p a n", p=128), [B_xgt[e % 3]], R=B_xgs)

        def transposes(e, part=None):
            i = e % 2
            xg_, Bxg_ = xgt[e % 3], B_xgt[e % 3]
            parts = range(6) if part is None else [part]
            for pt in parts:
                a = pt // 2
                pb_ = a % 2
                for kc in range(4 * (pt % 2), 4 * (pt % 2) + 4):
                    S.op("pe", lambda t: t.transpose(PSb[pb_][:, kc * 128:(kc + 1) * 128], xg_[:, a, kc * 128:(kc + 1) * 128], ident_b),
                         [Bxg_, B_cb], [BP[6 + pb_]], signal=(kc == 7))
                if pt % 2 == 1:
                    CP("act", xgT[i][:, :, a * 128:(a + 1) * 128], PSb[pb_].rearrange("p (k s) -> p k s", s=128), [BP[6 + pb_]], [B_xgT[i]])

        mctr = [0]

        def gate_up(e):
            i = e % 2
            pend = None

            def finish(m, j2):
                STT(hidT[i][:, m, :], ut[j2][:], 1.0, sg[j2][:], ALU.add, ALU.mult, [B_sg[j2], B_ut[j2]], [B_hid[i][m]])

            for m in range(8):
                j2 = mctr[0] % 2
                bg = 2 * j2
                bu = bg + 1
                mctr[0] += 1
                for kc in range(8):
                    MM(PS[:, bg, 0:CAP], wgu[i][:, kc, m * 128:(m + 1) * 128], xgT[i][:, kc, :], kc == 0, kc == 7, [B_wgu[i], B_xgT[i]], [BP[bg]])
                for kc in range(8):
                    MM(PS[:, bu, 0:CAP], wgu[i][:, kc, D + m * 128:D + (m + 1) * 128], xgT[i][:, kc, :], kc == 0, kc == 7, [B_wgu[i], B_xgT[i]], [BP[bu]])
                if m >= 2 and e + 1 < NE:
                    transposes(e + 1, part=m - 2)
                TS("dve", gt[j2][:], PS[:, bg, 0:CAP], bgu[:, e, m:m + 1], 7.0, ALU.add, ALU.min, [BP[bg], B_bias], [B_gt[j2]])
                ACT(sg[j2][:], gt[j2][:], AF.Gelu_apprx_sigmoid, [B_gt[j2]], [B_sg[j2]])
                TS("dve", ut[j2][:], PS[:, bu, 0:CAP], bgu[:, e, 8 + m:9 + m], None, ALU.add, ALU.bypass, [BP[bu], B_bias], [B_ut[j2]])
                TS("pool", ut[j2][:], ut[j2][:], 7.0, -7.0, ALU.min, ALU.max, [B_ut[j2]], [B_ut[j2]])
                if pend is not None:
                    finish(*pend)
                pend = (m, j2)
            finish(*pend)

        ectr = [0]

        def down(e):
            i = e % 2
            for a in range(3):
                j2 = ectr[0] % 2
                ectr[0] += 1
                b0 = 4 if a % 2 == 0 else 6
                for hf in range(2):
                    bk = b0 + hf
                    for kc in range(8):
                        MM(PS[:, bk, :], hidT[i][:, kc, a * 128:(a + 1) * 128], wdn[i][:, kc, hf * 512:(hf + 1) * 512], kc == 0, False, [B_hid[i][kc], B_wdn[i]], [BP[bk]])
                    MM(PS[:, bk, :], selb[:, e * 128:(e + 1) * 128], bdn[:, hf * 512:(hf + 1) * 512], False, True, [B_bias], [BP[bk]])
                CP("act", eot[j2][:], PS[:, b0:b0 + 2, :].rearrange("p a b -> p (a b)"), [BP[b0], BP[b0 + 1]], [B_eot[j2]])
                S.dma("sp", lambda g: g.dma_start(out=eo_d[e * CAP + a * 128:e * CAP + (a + 1) * 128, :], in_=eot[j2][:]), [B_eot[j2]], [B_eo[e]])

        load_tokens(0)
        load_tokens(1)
        load_expert(0)
        transposes(0)
        for e in range(NE):
            if e + 1 < NE:
                load_expert(e + 1)
            if e + 2 < NE:
                load_tokens(e + 2)
            gate_up(e)
            down(e)
        S.barrier()

        l2g = sb(st3, "l2g", [128, D], F32)
        l2b = sb(st3, "l2b", [128, D], F32)
        B_l2 = Buf("ln2")
        LD("sp", l2g[:], lnp_d[2], [B_l2])
        LD("sp", l2b[:], lnp_d[3], [B_l2])
        NG = 3
        gat = [[sb(st3, f"gat{i}_{k}", [128, D], F32) for k in range(4)] for i in range(NG)]
        B_gat = [[Buf(f"gat{i}_{k}") for k in range(4)] for i in range(NG)]
        x1t = [sb(st3, f"x1t{i}", [128, D], F32) for i in range(NG)]
        B_x1t = [Buf(f"x1t{i}") for i in range(NG)]
        st6b = [sb(st3, f"st6b{i}", [128, 2, 6], F32) for i in range(2)]
        mvb = [sb(st3, f"mvb{i}", [128, 2], F32) for i in range(2)]
        rsb = [sb(st3, f"rsb{i}", [128, 1], F32) for i in range(2)]
        nbb = [sb(st3, f"nbb{i}", [128, 1], F32) for i in range(2)]
        B_stb = [Buf("stb0"), Buf("stb1")]

        def fetch(tt):
            i3 = tt % NG
            LD("sp", x1t[i3][:], x1_d[tt * 128:(tt + 1) * 128, :], [B_x1t[i3]], R=[B_x1d[tt]])
            for k in range(4):
                S.dma("pool", lambda g: g.indirect_dma_start(out=gat[i3][k][:], out_offset=None, in_=eo_d,
                                                              in_offset=bass.IndirectOffsetOnAxis(ap=dstg2[:, tt * 4 + k:tt * 4 + k + 1], axis=0),
                                                              bounds_check=reg_ga, oob_is_err=False),
                      B_eo + [B_route[tt]], [B_gat[i3][k]])

        fetch(0)
        fetch(1)
        for tt in range(16):
            i3 = tt % NG
            i2 = tt % 2
            if tt + 2 < 16:
                fetch(tt + 2)
            acc = x1t[i3]
            Ba = B_x1t[i3]
            ACT(acc[:], acc[:], AF.Copy, [Ba], [Ba], scale=ALPHA)
            for k in range(4):
                STT(acc[:], gat[i3][k][:], wk[:, tt, k:k + 1], acc[:], ALU.mult, ALU.add, [B_gat[i3][k], B_route[tt], Ba], [Ba])
            for hf in range(2):
                S.op("dve", lambda v: v.bn_stats(out=st6b[i2][:, hf, :], in_=acc[:, hf * 512:(hf + 1) * 512]), [Ba], [B_stb[i2]])
            S.op("dve", lambda v: v.bn_aggr(out=mvb[i2][:], in_=st6b[i2][:].rearrange("p a b -> p (a b)")), [B_stb[i2]], [B_stb[i2]])
            ACT(rsb[i2][:], mvb[i2][:, 1:2], AF.Sqrt, [B_stb[i2]], [B_stb[i2]], scale=1.0, bias=LN_EPS)
            S.op("dve", lambda v: v.reciprocal(out=rsb[i2][:], in_=rsb[i2][:]), [B_stb[i2]], [B_stb[i2]])
            STT(nbb[i2][:], mvb[i2][:, 0:1], -1.0, rsb[i2][:], ALU.mult, ALU.mult, [B_stb[i2]], [B_stb[i2]])
            ACT(acc[:], acc[:], AF.Identity, [Ba, B_stb[i2]], [Ba], scale=rsb[i2][:, 0:1], bias=nbb[i2][:, 0:1])
            TT("dve", acc[:], acc[:], l2g[:], ALU.mult, [Ba, B_l2], [Ba])
            TT("pool", acc[:], acc[:], l2b[:], ALU.add, [Ba, B_l2], [Ba])
            S.dma("sp", lambda g: g.dma_start(out=out_d[tt * 128:(tt + 1) * 128, :], in_=acc[:]), [Ba], [], is_output=True)
        S.finish()
    return nc


def _consts(h):
    c = np.zeros((128, C_END), np.float32)
    c[:, C_ID:C_ID + 128] = np.eye(128, dtype=np.float32)
    c[:, C_ONE:C_ONE + 128] = 1.0
    k = np.arange(128)[:, None] // 64
    q = np.arange(128)[None, :] // 64
    c[:, C_MOWN:C_MOWN + 128] = (k <= q)
    c[:, C_MOTH:C_MOTH + 128] = (k <= q) if h == 1 else (k < q)
    c[:, C_TRI:C_TRI + 128] = (np.arange(128)[:, None] < np.arange(128)[None, :])
    c[:, C_IOTA:C_IOTA + 32] = np.arange(32, dtype=np.float32)[None, :]
    inv_freq = (10000.0 ** (-np.arange(0, 64, 2, dtype=np.float32) / np.float32(64))).astype(np.float32)
    c[0:64, C_INVF] = np.tile(inv_freq.astype(np.float64) / (2 * np.pi), 2).astype(np.float32)
    c[0:32, C_SGN] = -1.0
    c[32:64, C_SGN] = 1.0
    return c


def _prep_shared(inp):
    f = lambda a: np.ascontiguousarray(a, dtype=np.float32)
    w_in = inp["w_in"][0]
    o = np.cumsum([0, 1024, 1024, 1024, 384, 256, 64, 1024, 1024])
    gb, gc, hc, ql, kv, kpe, gcv, gat = [w_in[:, o[i]:o[i + 1]] for i in range(8)]
    sw = np.concatenate([np.arange(32, 64), np.arange(0, 32)])
    sh = {}
    sh["w_inA"] = f(np.concatenate([np.concatenate([gb[:, i * 128:(i + 1) * 128], gc[:, i * 128:(i + 1) * 128], hc[:, i * 128:(i + 1) * 128]], 1) for i in range(8)], 1))
    sh["w_inQ"] = f(ql)
    sh["w_inK"] = f(np.concatenate([kv, kpe, kpe[:, sw]], 1))
    wcb, wab = inp["w_conv_branch"][0], inp["w_attn_branch"][0]
    sh["w_mrg"] = f(np.concatenate([np.concatenate([wcb[:, i * 128:(i + 1) * 128], wab[:, i * 128:(i + 1) * 128], gcv[:, i * 128:(i + 1) * 128], gat[:, i * 128:(i + 1) * 128]], 1) for i in range(8)], 1))
    wuq = inp["w_uq"][0].reshape(384, 8, 192)
    sh["w_uq"] = f(np.concatenate([wuq[:, :, 0:128].reshape(384, 1024), wuq[:, :, 128:192].reshape(384, 512), wuq[:, :, 128:192][:, :, sw].reshape(384, 512)], 1))
    sh["w_uk"] = f(inp["w_uk"][0])
    sh["w_uv"] = f(inp["w_uv"][0])
    sh["w_out"] = f(inp["w_out"][0])
    sh["w_gu"] = f(inp["w_gate_up"][0])
    sh["w_dn"] = f(inp["w_down"][0])
    sh["cw"] = f(inp["conv_w"][0].T.reshape(8, 128, 3).transpose(1, 0, 2))
    sh["gq"] = f(inp["q_norm_g"][0].reshape(3, 128).T)
    sh["gkv"] = f(inp["kv_norm_g"][0].reshape(2, 128).T)
    sh["lnp"] = f(np.stack([np.broadcast_to(inp[k][0][None, :], (128, D)) for k in ("ln1_g", "ln1_b", "ln2_g", "ln2_b")]))
    sh["w_r"] = f(inp["w_router"][0].reshape(8, 128, NE).transpose(1, 0, 2))
    sh["b_r"] = f(np.broadcast_to(inp["b_router"][0][None, :], (128, NE)))
    sh["b_gu"] = f(inp["b_gate_up"][0].reshape(NE, 16, 128).transpose(2, 0, 1))
    sh["b_dn"] = f(inp["b_down"][0])
    sel = np.zeros((NE, NE, 128), np.float32)
    sel[np.arange(NE), np.arange(NE), :] = 1.0
    sh["selc"] = f(sel.reshape(NE, NE * 128))
    return sh


def _own_tokens(h):
    ch = 2 * np.arange(32) + h
    return (ch[:, None] * 64 + np.arange(64)[None, :]).ravel()


def _prep_core(inp, c):
    b, h = c // 2, c % 2
    xb = np.asarray(inp["x"][b], dtype=np.float32)
    own, oth = _own_tokens(h), _own_tokens(1 - h)
    halo = ((2 * np.arange(32) + h)[:, None] * 64 + np.array([-2, -1])[None, :]).ravel()
    xh = xb[np.maximum(halo, 0)].copy()
    xh[halo < 0] = 0.0
    p = np.asarray(inp["positions"][b]).astype(np.int32)
    d = {
        "xT_own": np.ascontiguousarray(xb[own].T),
        "xT_oth": np.ascontiguousarray(xb[oth].T),
        "xT_halo": np.ascontiguousarray(xh.T),
        "x_own": np.ascontiguousarray(xb[own]),
        "pos": np.ascontiguousarray(np.broadcast_to(np.concatenate([p[own], p[oth]])[None, :], (64, 2 * T))).astype(np.int32),
        "cst": _consts(h),
    }
    return d


_NC_CACHE = {}


def kernel(**inputs):
    inputs = {k: np.asarray(v) for k, v in inputs.items()}
    if "prog" not in _NC_CACHE:
        _NC_CACHE["prog"] = build_program()
    nc = _NC_CACHE["prog"]
    sh = _prep_shared(inputs)
    in_maps = []
    for c in range(NCORES):
        d = _prep_core(inputs, c)
        d.update(sh)
        in_maps.append(d)
    res = run_bass_kernel_spmd(nc, in_maps, core_ids=list(range(NCORES)))
    out = np.zeros((4, 4096, D), np.float32)
    for c in range(NCORES):
        out[c // 2, _own_tokens(c % 2), :] = res.results[c]["out"]
    return out
```

```python
from contextlib import ExitStack
import numpy as np
import concourse.bass as bass
import concourse.mybir as mybir
from concourse.bass_utils import run_bass_kernel_spmd

F32 = mybir.dt.float32
BF16 = mybir.dt.bfloat16
I32 = mybir.dt.int32
U32 = mybir.dt.uint32
AF = mybir.ActivationFunctionType
ALU = mybir.AluOpType
AX = mybir.AxisListType

NCORES = 8
D = 1024
T = 2048
SEG = 512
NSEG = 4
NE = 32
CAP = 384
NSLOT = NE * CAP
ALPHA = float(2.0 ** 0.25)
LN_EPS = 1e-5
RMS_EPS = 1e-6
QK_SCALE = float(192.0 ** -0.5)
MAGIC = 12582912.0
TWO_PI_S = float(2 * np.pi * (1 - 1e-6))

C_ID, C_ONE, C_MOWN, C_MOTH, C_TRI, C_IOTA, C_INVF, C_SGN, C_END = 0, 128, 256, 384, 512, 640, 672, 673, 674


class Ev:
    __slots__ = ("sem", "val", "eng")

    def __init__(self, sem, val, eng):
        self.sem, self.val, self.eng = sem, val, eng


class Buf:
    __slots__ = ("name", "w", "r")

    def __init__(self, name=""):
        self.name, self.w, self.r = name, None, {}


class Sched:
    SEM_LIMIT = 30000

    def __init__(self, nc, stack, n_dma_sems=64):
        self.nc, self.stack = nc, stack
        self.engs = {"pe": nc.tensor, "dve": nc.vector, "act": nc.scalar, "pool": nc.gpsimd, "sp": nc.sync}
        self.sem, self.cnt, self.waited, self.pending, self.last = {}, {}, {}, {}, {}
        self.nsem = 0
        for k in self.engs:
            self.sem[k] = self._newsem(k)
            self.cnt[k] = 0
            self.waited[k] = {}
            self.pending[k] = []
            self.last[k] = None
        self.dma_sems = [self._newsem("dma") for _ in range(n_dma_sems)]
        self.dma_cnt = [0] * n_dma_sems
        self.dma_last = [None] * n_dma_sems
        self.n_sw = (n_dma_sems * 5) // 8
        self.dma_i = {"sw": 0, "hw": 0}
        self.out_events = []

    def _newsem(self, tag):
        self.nsem += 1
        return self.stack.enter_context(self.nc.semaphore(f"s_{tag}_{self.nsem}"))

    def _wait(self, e, ev):
        if ev is None:
            return
        if ev.eng == e and e == "pe":
            return
        if ev.val is None:
            raise RuntimeError("waiting on unsignalled event")
        w = self.waited[e]
        key = id(ev.sem)
        if w.get(key, 0) >= ev.val:
            return
        self.engs[e].wait_ge(ev.sem, ev.val)
        w[key] = ev.val

    def _deps(self, e, reads, writes):
        for b in reads:
            self._wait(e, b.w)
        for b in writes:
            self._wait(e, b.w)
            for r in list(b.r.values()):
                if isinstance(r, list):
                    for x in r:
                        self._wait(e, x)
                else:
                    self._wait(e, r)

    def _mark(self, ev, reads, writes):
        for b in writes:
            b.w = ev
            b.r = {}
        for b in reads:
            if b not in writes:
                if ev.eng == "dma":
                    b.r.setdefault("dma", []).append(ev)
                else:
                    b.r[ev.eng] = ev

    def op(self, e, fn, reads=(), writes=(), signal=True):
        self._deps(e, reads, writes)
        ins = fn(self.engs[e])
        ev = Ev(None, None, e)
        if signal:
            if self.cnt[e] >= self.SEM_LIMIT:
                self.sem[e] = self._newsem(e)
                self.cnt[e] = 0
            self.cnt[e] += 1
            ins.then_inc(self.sem[e], 1)
            ev.sem, ev.val = self.sem[e], self.cnt[e]
            for p in self.pending[e]:
                p.sem, p.val = ev.sem, ev.val
            self.pending[e] = []
            self.last[e] = ev
        else:
            self.pending[e].append(ev)
        self._mark(ev, reads, writes)
        return ev

    def dma(self, q, fn, reads=(), writes=(), is_output=False):
        self._deps(q, reads, writes)
        if q == "pool":
            i = self.dma_i["sw"]
            self.dma_i["sw"] = (i + 1) % self.n_sw
        else:
            i = self.n_sw + self.dma_i["hw"]
            self.dma_i["hw"] = (self.dma_i["hw"] + 1) % (len(self.dma_sems) - self.n_sw)
        prev = self.dma_last[i]
        if prev is not None:
            w = self.waited[q]
            if w.get(id(prev.sem), 0) < prev.val:
                self.engs[q].wait_ge(prev.sem, prev.val)
                w[id(prev.sem)] = prev.val
        ins = fn(self.engs[q])
        self.dma_cnt[i] += 16
        ins.then_inc(self.dma_sems[i], 16)
        ev = Ev(self.dma_sems[i], self.dma_cnt[i], "dma")
        self.dma_last[i] = ev
        self._mark(ev, reads, writes)
        if is_output:
            self.out_events.append(ev)
        return ev

    def barrier(self):
        evs = [v for v in self.last.values() if v is not None] + [d for d in self.dma_last if d is not None]
        for e in self.engs:
            if self.pending[e]:
                raise RuntimeError("barrier with unsignalled instructions on " + e)
            for ev in evs:
                if ev.eng != e:
                    self._wait(e, ev)

    def finish(self):
        for ev in self.out_events:
            self._wait("sp", ev)
        for ev in self.dma_last:
            if ev is not None:
                self._wait("sp", ev)


def build_program(stage=9):
    nc = bass.Bass("TRN2", target_bir_lowering=False)

    def din(name, shape, dt=F32):
        return nc.dram_tensor(name, list(shape), dt, kind="ExternalInput").ap()

    xT_own_d = din("xT_own", [D, T])
    xT_oth_d = din("xT_oth", [D, T])
    xT_halo_d = din("xT_halo", [D, 64])
    x_own_d = din("x_own", [T, D])
    pos_d = din("pos", [64, 2 * T], I32)
    cst_d = din("cst", [128, C_END])
    sel_d = din("selc", [32, NE * 128])
    cw_d = din("cw", [128, 8, 3])
    gq_d = din("gq", [128, 3])
    gkv_d = din("gkv", [128, 2])
    lnp_d = din("lnp", [4, 128, D])
    wr_d = din("w_r", [128, 8, NE])
    br_d = din("b_r", [128, NE])
    bgu_d = din("b_gu", [128, NE, 16])
    bdn_d = din("b_dn", [NE, D])
    w_inA_d = din("w_inA", [D, 3072])
    w_inQ_d = din("w_inQ", [D, 384])
    w_inK_d = din("w_inK", [D, 384])
    w_mrg_d = din("w_mrg", [D, 4096])
    w_uq_d = din("w_uq", [384, 2048])
    w_uk_d = din("w_uk", [256, 1024])
    w_uv_d = din("w_uv", [256, 1024])
    w_out_d = din("w_out", [D, D])
    w_gu_d = din("w_gu", [NE, D, 2 * D])
    w_dn_d = din("w_dn", [NE, D, D])
    out_d = nc.dram_tensor("out", [T, D], F32, kind="ExternalOutput").ap()
    xg_d = nc.dram_tensor("xg_scr", [NSLOT, D], BF16, kind="Internal").ap()
    eo_d = nc.dram_tensor("eo_scr", [NSLOT + 128, D], F32, kind="Internal").ap()
    x1_d = nc.dram_tensor("x1_scr", [T, D], F32, kind="Internal").ap()

    def kp(ap):
        return ap.rearrange("(k p) n -> p k n", p=128)

    with ExitStack() as st0x:
        st0 = st0x
        S = Sched(nc, st0x)

        dsts2 = st0.enter_context(nc.sbuf_tensor("dsts", [128, 64], I32))
        dstg2 = st0.enter_context(nc.sbuf_tensor("dstg", [128, 64], I32))
        reg_sc = nc.gpsimd.alloc_register("bc_scatter")
        nc.gpsimd.reg_mov(reg_sc, NSLOT - 1)
        reg_ga = nc.gpsimd.alloc_register("bc_gather")
        nc.gpsimd.reg_mov(reg_ga, NSLOT + 127)
        ARENA_BYTES = 207 * 1024
        arena = st0.enter_context(nc.sbuf_tensor("arena", [128, ARENA_BYTES // 2], BF16))
        DTSZ = {F32: 4, BF16: 2, I32: 4, U32: 4}

        class Region:
            def __init__(self, base_kb, limit_kb):
                self.ptr, self.limit = int(base_kb * 1024), int(limit_kb * 1024)

            def alloc(self, shape, dt):
                n = int(np.prod(shape[1:])) * DTSZ[dt]
                n = (n + 63) // 64 * 64
                off = self.ptr
                self.ptr += n
                if self.ptr > self.limit:
                    raise RuntimeError(f"region overflow {self.ptr} > {self.limit}")
                nel = int(np.prod(shape[1:]))
                v = arena[0:shape[0], off // 2: off // 2 + nel * DTSZ[dt] // 2]
                if dt != BF16:
                    v = v.bitcast(dt)
                if len(shape) == 3:
                    v = v.rearrange("p (a b) -> p a b", a=shape[1])
                return v

        def sb(reg, name, shape, dt):
            return reg.alloc(list(shape), dt)

        st0 = Region(0, 6)
        P1 = Region(6, 70)
        KQ = Region(70, 122)
        RX = Region(122, 154)
        W1a = Region(154, 166)
        TKQ = Region(166, 207)
        TA = Region(166, 207)
        st1b = Region(122, 207)
        st1c = Region(70, 207)
        st2 = Region(102, 207)
        st3 = Region(6, 207)

        def MM(out, lhsT, rhs, st, sp, R, W, sig=None):
            S.op("pe", lambda t: t.matmul(out, lhsT=lhsT, rhs=rhs, start=st, stop=sp), R, W,
                 signal=(sp if sig is None else sig))

        def ACT(out, in_, func, R, W, **kw):
            S.op("act", lambda a: a.activation(out=out, in_=in_, func=func, **kw), R, W)

        def TT(e, out, a, b, op, R, W):
            S.op(e, lambda v: v.tensor_tensor(out=out, in0=a, in1=b, op=op), R, W)

        def TS(e, out, a, s1, s2, op0, op1, R, W):
            S.op(e, lambda v: v.tensor_scalar(out=out, in0=a, scalar1=s1, scalar2=s2, op0=op0, op1=op1), R, W)

        def STT(out, a, s, b, op0, op1, R, W, **kw):
            S.op("dve", lambda v: v.scalar_tensor_tensor(out=out, in0=a, scalar=s, in1=b, op0=op0, op1=op1, **kw), R, W)

        def CP(e, out, in_, R, W):
            if e == "act":
                S.op(e, lambda a: a.activation(out=out, in_=in_, func=AF.Copy), R, W)
            else:
                S.op(e, lambda v: v.tensor_copy(out=out, in_=in_), R, W)

        def LD(q, out, in_, W, R=()):
            return S.dma(q, lambda g: g.dma_start(out=out, in_=in_), reads=R, writes=W)

        PS = st0x.enter_context(nc.psum_tensor("PS", [128, 8, 512], F32))
        BP = [Buf(f"ps{i}") for i in range(8)]

        cst = sb(st0, "cst", [128, C_END], F32)
        B_c = Buf("cst")
        LD("sp", cst[:], cst_d, [B_c])
        cbf = sb(st0, "cbf", [128, C_IOTA], BF16)
        B_cb = Buf("cbf")
        CP("dve", cbf[:], cst[:, 0:C_IOTA], [B_c], [B_cb])
        ident_f = cst[:, C_ID:C_ID + 128]
        ones_f = cst[:, C_ONE:C_ONE + 128]
        ident_b = cbf[:, C_ID:C_ID + 128]
        ones_b = cbf[:, C_ONE:C_ONE + 128]
        mask_b = [cbf[:, C_MOWN:C_MOWN + 128], cbf[:, C_MOTH:C_MOTH + 128]]
        tri_b = cbf[:, C_TRI:C_TRI + 128]
        iota_f = cst[:, C_IOTA:C_IOTA + 32]
        invf = cst[0:64, C_INVF:C_INVF + 1]
        sgn = cst[0:64, C_SGN:C_SGN + 1]
        dsts = dsts2[:].rearrange("p (a b) -> p a b", b=4)
        dstg = dstg2[:].rearrange("p (a b) -> p a b", b=4)
        wk = sb(st0, "wk", [128, 16, 4], F32)

        xTo = sb(RX, "xTo", [128, 8, T], BF16)
        B_xTo = [Buf(f"xTo{s}") for s in range(NSEG)]
        ckvT = sb(KQ, "ckvT", [128, 2, 2 * T], BF16)
        kpeT = sb(KQ, "kpeT", [64, 2 * T], BF16)
        B_kv = [Buf(f"kv{s}") for s in range(8)]
        qlT = sb(KQ, "qlT", [128, 3, T], BF16)
        B_ql = [Buf(f"ql{s}") for s in range(NSEG)]
        ycT = sb(P1, "ycT", [128, 8, T], BF16)
        B_yc = [Buf(f"yc{f}") for f in range(8)]
        cso = sb(KQ, "cso", [64, T], F32)
        sno = sb(KQ, "sno", [64, T], F32)
        B_cs = [Buf(f"cs{s}") for s in range(NSEG)]
        attnT = sb(P1, "attnT", [128, 8, T], BF16)
        B_at = [[Buf(f"at{h}_{j}") for j in range(NSEG)] for h in range(8)]

        st1a = TKQ
        wsl = [sb(W1a, f"wsl{i}", [128, 8, 384], BF16) for i in range(2)]
        B_wsl = [Buf("wsl0"), Buf("wsl1")]
        xob0 = sb(st1a, "xob0", [128, 8, SEG], BF16)
        xob = [xob0, xob0]
        B_xob0 = Buf("xob0")
        B_xob = [B_xob0, B_xob0]
        posi = sb(st1a, "posi", [64, SEG], I32)
        B_pos = Buf("pos")
        gq = sb(st1a, "gq", [128, 3], F32)
        gkv = sb(st1a, "gkv", [128, 2], F32)
        B_gn = Buf("gains")
        LD("sp", gq[:], gq_d, [B_gn])
        LD("sp", gkv[:], gkv_d, [B_gn])
        raw = sb(st1a, "raw", [128, 3, SEG], F32)
        sq = sb(st1a, "sq", [128, 3, SEG], F32)
        B_raw, B_sq = Buf("raw"), Buf("sq")
        rk = sb(st1a, "rk", [128, SEG], F32)
        B_rk = Buf("rk")
        tA = sb(st1a, "tA", [64, SEG], F32)
        tB = sb(st1a, "tB", [64, SEG], F32)
        B_tA, B_tB = Buf("tA"), Buf("tB")
        cst_t = sb(st1a, "cst_t", [64, SEG], F32)
        snt_t = sb(st1a, "snt_t", [64, SEG], F32)
        B_cst = Buf("cst_t")
        rp1 = sb(st1a, "rp1", [64, SEG], F32)
        rp2 = sb(st1a, "rp2", [64, SEG], F32)
        rp3 = sb(st1a, "rp3", [64, SEG], F32)
        B_rp = Buf("rp")

        def rope_tables(col0, cs_out, sn_out, Bout):
            LD("sp", posi[:], pos_d[:, col0:col0 + SEG], [B_pos])
            CP("dve", rp1[:], posi[:], [B_pos], [B_rp])
            TS("dve", rp1[:], rp1[:], invf, None, ALU.mult, ALU.bypass, [B_c, B_rp], [B_rp])
            TS("dve", rp2[:], rp1[:], MAGIC, MAGIC, ALU.add, ALU.subtract, [B_rp], [B_rp])
            TT("dve", rp2[:], rp1[:], rp2[:], ALU.subtract, [B_rp], [B_rp])
            ACT(sn_out, rp2[:], AF.Sin, [B_rp], [Bout], scale=TWO_PI_S)
            TS("dve", sn_out, sn_out, sgn, None, ALU.mult, ALU.bypass, [B_c, Bout], [Bout])
            TS("dve", rp1[:], rp1[:], 0.25, None, ALU.add, ALU.bypass, [B_rp], [B_rp])
            TS("dve", rp3[:], rp1[:], MAGIC, MAGIC, ALU.add, ALU.subtract, [B_rp], [B_rp])
            TT("dve", rp3[:], rp1[:], rp3[:], ALU.subtract, [B_rp], [B_rp])
            ACT(cs_out, rp3[:], AF.Sin, [B_rp], [Bout], scale=TWO_PI_S)

        LD("pool", wsl[0][:], kp(w_inK_d), [B_wsl[0]])
        for s in range(NSEG):
            LD("pool", xTo[:, :, s * SEG:(s + 1) * SEG], kp(xT_own_d[:, s * SEG:(s + 1) * SEG]), [B_xTo[s]])
            if s == 0:
                LD("pool", wsl[1][:], kp(w_inQ_d), [B_wsl[1]])
        korder = [0, 4, 1, 5, 2, 6, 3, 7]

        def kseg(s):
            if s < NSEG:
                return (xTo[:, :, s * SEG:(s + 1) * SEG], B_xTo[s], cso[:, s * SEG:(s + 1) * SEG], sno[:, s * SEG:(s + 1) * SEG], B_cs[s])
            return (xob[0][:], B_xob[0], cst_t[:], snt_t[:], B_cst)

        rope_tables(korder[0] * SEG, *kseg(korder[0])[2:])
        for it, s in enumerate(korder):
            xs, Bx, cs_ap, sn_ap, Bt = kseg(s)
            if s >= NSEG:
                LD("pool", xob[0][:], kp(xT_oth_d[:, (s - 4) * SEG:(s - 3) * SEG]), [Bx])
            o = 4 * (it % 2)
            for c in range(2):
                for kc in range(8):
                    MM(PS[:, o + c, :], wsl[0][:, kc, c * 128:(c + 1) * 128], xs[:, kc, :], kc == 0, kc == 7, [B_wsl[0], Bx], [BP[o + c]])
            for c in range(2):
                for kc in range(8):
                    MM(PS[0:64, o + 2 + c, :], wsl[0][:, kc, 256 + c * 64:320 + c * 64], xs[:, kc, :], kc == 0, kc == 7, [B_wsl[0], Bx], [BP[o + 2 + c]])
            if it + 1 < 8:
                s2 = korder[it + 1]
                rope_tables(s2 * SEG, *kseg(s2)[2:])
            for c in range(2):
                ACT(raw[:, c, :], PS[:, o + c, :], AF.Copy, [BP[o + c]], [B_raw])
                ACT(sq[:, c, :], PS[:, o + c, :], AF.Square, [BP[o + c]], [B_sq])
            for c in range(2):
                MM(PS[:, o, :], ones_f, sq[:, c, :], c == 0, c == 1, [B_c, B_sq], [BP[o]])
            ACT(rk[:], PS[:, o, :], AF.Ln, [BP[o]], [B_rk], scale=1.0 / 256.0, bias=RMS_EPS)
            ACT(rk[:], rk[:], AF.Exp, [B_rk], [B_rk], scale=-0.5)
            for c in range(2):
                STT(ckvT[:, c, s * SEG:(s + 1) * SEG], raw[:, c, :], gkv[:, c:c + 1], rk[:], ALU.mult, ALU.mult, [B_raw, B_rk, B_gn], [B_kv[s]])
            TT("dve", tA[:], PS[0:64, o + 2, :], cs_ap, ALU.mult, [BP[o + 2], Bt], [B_tA])
            TT("dve", tB[:], PS[0:64, o + 3, :], sn_ap, ALU.mult, [BP[o + 3], Bt], [B_tB])
            TT("dve", kpeT[:, s * SEG:(s + 1) * SEG], tA[:], tB[:], ALU.add, [B_tA, B_tB], [B_kv[s]])

        for s in range(NSEG):
            xs, Bx = xTo[:, :, s * SEG:(s + 1) * SEG], B_xTo[s]
            o = 4 * (s % 2)
            for c in range(3):
                for kc in range(8):
                    MM(PS[:, o + c, :], wsl[1][:, kc, c * 128:(c + 1) * 128], xs[:, kc, :], kc == 0, kc == 7, [B_wsl[1], Bx], [BP[o + c]])
            for c in range(3):
                ACT(raw[:, c, :], PS[:, o + c, :], AF.Copy, [BP[o + c]], [B_raw])
                ACT(sq[:, c, :], PS[:, o + c, :], AF.Square, [BP[o + c]], [B_sq])
            for c in range(3):
                MM(PS[:, o + 3, :], ones_f, sq[:, c, :], c == 0, c == 2, [B_c, B_sq], [BP[o + 3]])
            ACT(rk[:], PS[:, o + 3, :], AF.Ln, [BP[o + 3]], [B_rk], scale=1.0 / 384.0, bias=RMS_EPS)
            ACT(rk[:], rk[:], AF.Exp, [B_rk], [B_rk], scale=-0.5)
            for c in range(3):
                STT(qlT[:, c, s * SEG:(s + 1) * SEG], raw[:, c, :], gq[:, c:c + 1], rk[:], ALU.mult, ALU.mult, [B_raw, B_rk, B_gn], [B_ql[s]])

        S.barrier()
        st1a = TA
        cw = sb(st1a, "cw", [128, 8, 3], F32)
        B_par = Buf("par1a")
        LD("sp", cw[:], cw_d, [B_par])
        xh = sb(st1a, "xh", [128, 8, 64], BF16)
        B_xh = Buf("xh")
        LD("pool", xh[:], kp(xT_halo_d), [B_xh])
        uext = sb(st1a, "uext", [128, 32, 66], F32)
        B_u = Buf("uext")
        gbs = sb(st1a, "gbs", [128, T], F32)
        B_gbs = Buf("gbs")
        gcs = [sb(st1a, f"gcs{i}", [128, SEG], F32) for i in range(2)]
        B_gcs = [Buf("gcs0"), Buf("gcs1")]
        yv = sb(st1a, "yv", [128, 32, 64], F32)
        B_yv = Buf("yv")
        for f in range(8):
            w_, Bw = wsl[f % 2], B_wsl[f % 2]
            LD("pool", w_[:], kp(w_inA_d[:, f * 384:(f + 1) * 384]), [Bw])
            for c in range(2):
                for kc in range(8):
                    MM(PS[:, 6 + c, 0:64], w_[:, kc, 128 + c * 128:256 + c * 128], xh[:, kc, :], kc == 0, kc == 7, [Bw, B_xh], [BP[6 + c]])
            CP("act", gcs[0][:, 0:64], PS[:, 6, 0:64], [BP[6]], [B_gcs[0]])
            TT("dve", uext[:, :, 0:2], gcs[0][:, 0:64].rearrange("p (c r) -> p c r", r=2), PS[:, 7, 0:64].rearrange("p (c r) -> p c r", r=2),
               ALU.mult, [B_gcs[0], BP[7]], [B_u])
            for s in range(NSEG):
                xs, Bx = xTo[:, :, s * SEG:(s + 1) * SEG], B_xTo[s]
                for c in range(3):
                    bk = 3 * (s % 2) + c
                    for kc in range(8):
                        MM(PS[:, bk, :], w_[:, kc, c * 128:(c + 1) * 128], xs[:, kc, :], kc == 0, kc == 7, [Bw, Bx], [BP[bk]])
                b0 = 3 * (s % 2)
                CP("act", gbs[:, s * SEG:(s + 1) * SEG], PS[:, b0, :], [BP[b0]], [B_gbs])
                CP("act", gcs[s % 2][:], PS[:, b0 + 1, :], [BP[b0 + 1]], [B_gcs[s % 2]])
                TT("dve", uext[:, 8 * s:8 * s + 8, 2:66], gcs[s % 2][:].rearrange("p (c r) -> p c r", r=64),
                   PS[:, b0 + 2, :].rearrange("p (c r) -> p c r", r=64), ALU.mult, [B_gcs[s % 2], BP[b0 + 2]], [B_u])
            TS("dve", yv[:], uext[:, :, 2:66], cw[:, f, 2:3], None, ALU.mult, ALU.bypass, [B_u, B_par], [B_yv])
            STT(yv[:], uext[:, :, 1:65], cw[:, f, 1:2], yv[:], ALU.mult, ALU.add, [B_u, B_par, B_yv], [B_yv])
            STT(yv[:], uext[:, :, 0:64], cw[:, f, 0:1], yv[:], ALU.mult, ALU.add, [B_u, B_par, B_yv], [B_yv])
            TT("dve", ycT[:, f, :], gbs[:], yv[:].rearrange("p c r -> p (c r)"), ALU.mult, [B_gbs, B_yv], [B_yc[f]])
        S.barrier()

        wuq = sb(st1b, "wuq", [128, 3, 2048], BF16)
        wuk = sb(st1b, "wuk", [128, 2, 1024], BF16)
        wuv = sb(st1b, "wuv", [128, 2, 1024], BF16)
        B_wu = Buf("wu")
        LD("pool", wuq[:], kp(w_uq_d), [B_wu])
        LD("pool", wuk[:], kp(w_uk_d), [B_wu])
        LD("pool", wuv[:], kp(w_uv_d), [B_wu])
        knT = [sb(st1b, f"knT{i}", [128, 2 * T], BF16) for i in range(2)]
        Vh = [sb(st1b, f"Vh{i}", [128, 32, 128], BF16) for i in range(2)]
        qn = [sb(st1b, f"qn{i}", [128, T], BF16) for i in range(2)]
        qr = [sb(st1b, f"qr{i}", [64, T], BF16) for i in range(2)]
        B_kn = [[Buf(f"kn{i}_{s}") for s in range(8)] for i in range(2)]
        B_V = [[Buf(f"V{i}_{g}") for g in range(8)] for i in range(2)]
        B_qn = [[Buf(f"qn{i}_{s}") for s in range(NSEG)] for i in range(2)]
        B_qr = [[Buf(f"qr{i}_{s}") for s in range(NSEG)] for i in range(2)]
        qa = sb(st1b, "qa", [64, SEG], F32)
        qb = sb(st1b, "qb", [64, SEG], F32)
        B_qa, B_qb = Buf("qa"), Buf("qb")
        NPT = 4
        pT = [sb(st1b, f"pT{i}", [128, SEG], BF16) for i in range(NPT)]
        B_pT = [Buf(f"pT{i}") for i in range(NPT)]
        rL = [sb(st1b, f"rL{i}", [128, SEG], F32) for i in range(2)]
        B_rL = [Buf("rL0"), Buf("rL1")]
        accL = [sb(st1b, f"accL{i}", [128, SEG], F32) for i in range(2)]
        B_acc = [Buf("accL0"), Buf("accL1")]
        B_xg = Buf("xg_d")
        zsrc = attnT[:, 4:8, :]
        B_zs = [B_at[hh][jj] for hh in range(4, 8) for jj in range(NSEG)]
        S.op("pool", lambda g: g.memset(zsrc, 0.0), [], B_zs)
        xgz = xg_d.rearrange("(p a) n -> p a n", p=128)
        zview = zsrc.rearrange("p a (b c) -> p (a b) c", c=D)
        for i in range(NSLOT // 128 // 8):
            S.dma("sp", lambda g: g.dma_start(out=xgz[:, 8 * i:8 * i + 8, :], in_=zview), B_zs, [B_xg])

        def prep_units(h):
            i = h % 2
            units = []

            def k_unit(s):
                def f():
                    bk = 6 + s % 2
                    for kc in range(2):
                        MM(PS[:, bk, :], wuk[:, kc, h * 128:(h + 1) * 128], ckvT[:, kc, s * SEG:(s + 1) * SEG], kc == 0, kc == 1, [B_wu, B_kv[s]], [BP[bk]])
                    CP("act", knT[i][:, s * SEG:(s + 1) * SEG], PS[:, bk, :], [BP[bk]], [B_kn[i][s]])
                return f

            def v_unit(g):
                def f():
                    bk = 6 + g % 2
                    for ii in range(4):
                        tt = 4 * g + ii
                        for kc in range(2):
                            MM(PS[:, bk, ii * 128:(ii + 1) * 128], ckvT[:, kc, tt * 128:(tt + 1) * 128], wuv[:, kc, h * 128:(h + 1) * 128],
                               kc == 0, kc == 1, [B_wu, B_kv[tt // 4]], [BP[bk]], sig=(kc == 1 and ii == 3))
                    CP("act", Vh[i][:, 4 * g:4 * g + 4, :].rearrange("p a b -> p (a b)"), PS[:, bk, :], [BP[bk]], [B_V[i][g]])
                return f

            def q_unit(s):
                def f():
                    cols = slice(s * SEG, (s + 1) * SEG)
                    for kc in range(3):
                        MM(PS[:, 6, :], wuq[:, kc, h * 128:(h + 1) * 128], qlT[:, kc, cols], kc == 0, kc == 2, [B_wu, B_ql[s]], [BP[6]])
                    for kc in range(3):
                        MM(PS[0:64, 7, :], wuq[:, kc, 1024 + h * 64:1088 + h * 64], qlT[:, kc, cols], kc == 0, kc == 2, [B_wu, B_ql[s]], [BP[7]])
                    CP("act", qn[i][:, cols], PS[:, 6, :], [BP[6]], [B_qn[i][s]])
                    TT("dve", qa[:], PS[0:64, 7, :], cso[:, cols], ALU.mult, [BP[7], B_cs[s]], [B_qa])
                    for kc in range(3):
                        MM(PS[0:64, 6, :], wuq[:, kc, 1536 + h * 64:1600 + h * 64], qlT[:, kc, cols], kc == 0, kc == 2, [B_wu, B_ql[s]], [BP[6]])
                    TT("dve", qb[:], PS[0:64, 6, :], sno[:, cols], ALU.mult, [BP[6], B_cs[s]], [B_qb])
                    TT("pool", qr[i][:, cols], qa[:], qb[:], ALU.add, [B_qa, B_qb], [B_qr[i][s]])
                return f

            for s in range(8):
                units.append(k_unit(s))
                units.append(v_unit(s))
            for s in range(NSEG):
                units.append(q_unit(s))
            order = [16, 0, 1, 8, 9, 17, 2, 3, 10, 11, 18, 4, 5, 12, 13, 19, 6, 7, 14, 15]
            return [units[k] for k in order]

        for u in prep_units(0):
            u()
        seg_ctr = 0
        tile_ctr = 0
        LOOK = 2
        for h in range(8):
            i = h % 2
            nxt = prep_units(h + 1) if h + 1 < 8 else []
            tiles = []
            for j in range(NSEG):
                tl = []
                for m in range(4):
                    tl.append((j * SEG + m * 128, m * 128, 0))
                    tl.append((T + j * SEG + m * 128, m * 128, 1))
                for js in range(j):
                    for m in range(4):
                        tl.append((js * SEG + m * 128, 0, None))
                        tl.append((T + js * SEG + m * 128, 0, None))
                tl.sort(key=lambda t: t[1])
                for ti, (k0, q0, mi) in enumerate(tl):
                    tiles.append((j, k0, q0, mi, ti == 0, ti == len(tl) - 1))
            nt = len(tiles)
            segbuf = {}
            for idx in range(nt + LOOK):
                if idx < nt:
                    j, k0, q0, mi, first, last = tiles[idx]
                    if first:
                        segbuf[j] = seg_ctr
                        seg_ctr += 1
                    bs = (tile_ctr + idx) % 3
                    pb = (tile_ctr + idx) % NPT
                    nq = SEG - q0
                    qc0 = j * SEG + q0
                    ks = k0 // SEG
                    MM(PS[:, bs, 0:nq], knT[i][:, k0:k0 + 128], qn[i][:, qc0:qc0 + nq], True, False, [B_kn[i][ks], B_qn[i][j]], [BP[bs]])
                    MM(PS[:, bs, 0:nq], kpeT[:, k0:k0 + 128], qr[i][:, qc0:qc0 + nq], False, True, [B_kv[ks], B_qr[i][j]], [BP[bs]])
                    ACT(pT[pb][:, 0:nq], PS[:, bs, 0:nq], AF.Exp, [BP[bs]], [B_pT[pb]], scale=QK_SCALE)
                    if mi is not None:
                        TT("dve", pT[pb][:, 0:128], pT[pb][:, 0:128], mask_b[mi], ALU.mult, [B_pT[pb], B_cb], [B_pT[pb]])
                    sc = segbuf[j] % 2
                    if first:
                        CP("dve", accL[sc][:], pT[pb][:], [B_pT[pb]], [B_acc[sc]])
                    else:
                        TT("dve", accL[sc][:, q0:SEG], accL[sc][:, q0:SEG], pT[pb][:, 0:nq], ALU.add, [B_acc[sc], B_pT[pb]], [B_acc[sc]])
                if idx >= LOOK:
                    j, k0, q0, mi, first, last = tiles[idx - LOOK]
                    pb = (tile_ctr + idx - LOOK) % NPT
                    nq = SEG - q0
                    sc = segbuf[j] % 2
                    bo = 3 + sc
                    MM(PS[:, bo, q0:SEG], Vh[i][:, k0 // 128, :], pT[pb][:, 0:nq], first, last, [B_V[i][k0 // SEG], B_pT[pb]], [BP[bo]], sig=True)
                    if last:
                        MM(PS[:, 5, :], ones_f, accL[sc][:], True, True, [B_c, B_acc[sc]], [BP[5]])
                        S.op("dve", lambda v: v.reciprocal(out=rL[sc][:], in_=PS[:, 5, :]), [BP[5]], [B_rL[sc]])
                        TT("dve", attnT[:, h, j * SEG:(j + 1) * SEG], PS[:, bo, :], rL[sc][:], ALU.mult, [BP[bo], B_rL[sc]], [B_at[h][j]])
                if nxt and idx % 4 == 3:
                    nxt.pop(0)()
            for u in nxt:
                u()
            tile_ctr += nt
        S.barrier()

        xTo = sb(st1c, "xTo2", [128, 8, T], BF16)
        B_xTo = [Buf(f"xTo2{s}") for s in range(NSEG)]
        mgT = sb(st1c, "mgT", [128, 8, T], BF16)
        B_mg = [Buf(f"mg{s}") for s in range(NSEG)]
        mark_wm = st1c.ptr
        wm = [sb(st1c, f"wm{i}", [128, 8, 512], BF16) for i in range(2)]
        B_wm = [Buf("wm0"), Buf("wm1")]
        wo = sb(st1c, "wo", [128, 8, D], BF16)
        B_wo = Buf("wo")
        LD("pool", wm[0][:], kp(w_mrg_d[:, 0:512]), [B_wm[0]])
        for s in range(NSEG):
            LD("pool", xTo[:, :, s * SEG:(s + 1) * SEG], kp(xT_own_d[:, s * SEG:(s + 1) * SEG]), [B_xTo[s]])
        LD("pool", wm[1][:], kp(w_mrg_d[:, 512:1024]), [B_wm[1]])
        LD("pool", wo[:], kp(w_out_d), [B_wo])
        mark_1c = st1c.ptr
        sgc = [sb(st1c, f"sgc{i}", [128, SEG], F32) for i in range(2)]
        sga = [sb(st1c, f"sga{i}", [128, SEG], F32) for i in range(2)]
        B_sgc = [Buf("sgc0"), Buf("sgc1")]
        B_sga = [Buf("sga0"), Buf("sga1")]
        all_at = [B_at[h][j] for h in range(8) for j in range(NSEG)]
        it = 0
        for f in range(8):
            w_, Bw = wm[f % 2], B_wm[f % 2]
            if 1 <= f < 7:
                LD("pool", wm[(f + 1) % 2][:], kp(w_mrg_d[:, (f + 1) * 512:(f + 2) * 512]), [B_wm[(f + 1) % 2]])
            for s in range(NSEG):
                b0 = 4 * (it % 2)
                i2 = it % 2
                it += 1
                cols = slice(s * SEG, (s + 1) * SEG)
                for kc in range(8):
                    MM(PS[:, b0, :], w_[:, kc, 0:128], ycT[:, kc, cols], kc == 0, kc == 7, [Bw, B_yc[kc]], [BP[b0]])
                for kc in range(8):
                    MM(PS[:, b0 + 1, :], w_[:, kc, 128:256], attnT[:, kc, cols], kc == 0, kc == 7, [Bw, B_at[kc][s]], [BP[b0 + 1]])
                for kc in range(8):
                    MM(PS[:, b0 + 2, :], w_[:, kc, 256:384], xTo[:, kc, cols], kc == 0, kc == 7, [Bw, B_xTo[s]], [BP[b0 + 2]])
                for kc in range(8):
                    MM(PS[:, b0 + 3, :], w_[:, kc, 384:512], xTo[:, kc, cols], kc == 0, kc == 7, [Bw, B_xTo[s]], [BP[b0 + 3]])
                ACT(sgc[i2][:], PS[:, b0 + 2, :], AF.Sigmoid, [BP[b0 + 2]], [B_sgc[i2]])
                ACT(sga[i2][:], PS[:, b0 + 3, :], AF.Sigmoid, [BP[b0 + 3]], [B_sga[i2]])
                TT("dve", sgc[i2][:], sgc[i2][:], PS[:, b0, :], ALU.mult, [B_sgc[i2], BP[b0]], [B_sgc[i2]])
                TT("dve", sga[i2][:], sga[i2][:], PS[:, b0 + 1, :], ALU.mult, [B_sga[i2], BP[b0 + 1]], [B_sga[i2]])
                TT("pool", mgT[:, f, cols], sgc[i2][:], sga[i2][:], ALU.add, [B_sgc[i2], B_sga[i2]], [B_mg[s]])

        lng = sb(st1c, "lng", [128, D], F32)
        lnb = sb(st1c, "lnb", [128, D], F32)
        B_ln = Buf("ln1")
        LD("sp", lng[:], lnp_d[0], [B_ln])
        LD("sp", lnb[:], lnp_d[1], [B_ln])
        wrt = sb(st1c, "wrt", [128, 8, NE], F32)
        brt = sb(st1c, "brt", [128, NE], F32)
        B_wr = Buf("wr")
        LD("sp", wrt[:], wr_d, [B_wr])
        LD("sp", brt[:], br_d, [B_wr])
        S.barrier()
        mark_2 = st1c.ptr
        st1c.ptr = mark_1c
        xt = [sb(st1c, f"xt{i}", [128, D], F32) for i in range(2)]
        B_xt = [Buf("xt0"), Buf("xt1")]
        st1c.ptr = max(st1c.ptr, mark_2)
        NH, LA = 5, 4
        h1 = [sb(st1c, f"h1{i}", [128, D], F32) for i in range(2)]
        Rwm = Region(mark_wm / 1024.0, mark_wm / 1024.0 + 16)
        h1 += [sb(Rwm, f"h1{i}", [128, D], F32) for i in range(2, NH)]
        B_h1 = [Buf(f"h1{i}") for i in range(NH)]
        xlo = [sb(Rwm, f"xlo{i}", [128, D], BF16) for i in range(2)]
        B_xlo = [Buf("xlo0"), Buf("xlo1")]
        x1b_all = xTo.rearrange("p a (b c) -> p (a b) c", c=D)
        B_x1b = [Buf(f"x1b{t}") for t in range(16)]
        x1Th = sb(st1c, "x1Th", [128, 8, 128], BF16)
        x1Tl = sb(st1c, "x1Tl", [128, 8, 128], BF16)
        B_x1T = Buf("x1T")
        wrh = sb(st1c, "wrh", [128, 8, NE], BF16)
        wrl = sb(st1c, "wrl", [128, 8, NE], BF16)
        wrd = sb(st1c, "wrd", [128, 8, NE], F32)
        B_wrs = Buf("wr_split")
        CP("dve", wrh[:], wrt[:], [B_wr], [B_wrs])
        TT("dve", wrd[:], wrt[:], wrh[:], ALU.subtract, [B_wr, B_wrs], [B_wrs])
        CP("dve", wrl[:], wrd[:], [B_wrs], [B_wrs])
        st6 = [sb(st1c, f"st6{i}", [128, 2, 6], F32) for i in range(2)]
        mv = [sb(st1c, f"mv{i}", [128, 2], F32) for i in range(2)]
        rs = [sb(st1c, f"rs{i}", [128, 1], F32) for i in range(2)]
        nb = [sb(st1c, f"nb{i}", [128, 1], F32) for i in range(2)]
        B_st = [Buf("st0"), Buf("st1")]
        mhalf = sb(st1c, "mhalf", [128, 1], F32)
        B_mh = Buf("mhalf")
        S.op("pool", lambda g: g.memset(mhalf[:], -0.5), [], [B_mh])
        lgt_all = sb(st1c, "lgt_all", [128, 16, NE], F32)
        top8_all = sb(st1c, "top8_all", [128, 16, 8], F32)
        idx8_all = sb(st1c, "idx8_all", [128, 16, 8], U32)
        B_lg = [Buf(f"lg{t}") for t in range(16)]
        B_route = [Buf(f"route{t}") for t in range(16)]
        B_x1d = [Buf(f"x1d{t}") for t in range(16)]

        def stageA(tt):
            s = tt // 4
            i2 = tt % 2
            i3 = tt % NH
            tcols = slice(tt * 128, (tt + 1) * 128)
            LD("sp", xt[i2][:], x_own_d[tt * 128:(tt + 1) * 128, :], [B_xt[i2]])
            for hf in range(2):
                for kc in range(8):
                    MM(PS[:, 2 * i2 + hf, :], mgT[:, kc, tcols], wo[:, kc, hf * 512:(hf + 1) * 512], kc == 0, kc == 7, [B_mg[s], B_wo], [BP[2 * i2 + hf]])
            psv = PS[:, 2 * i2:2 * i2 + 2, :].rearrange("p a b -> p (a b)")
            STT(h1[i3][:], xt[i2][:], ALPHA, psv, ALU.mult, ALU.add, [B_xt[i2], BP[2 * i2], BP[2 * i2 + 1]], [B_h1[i3]])
            for hf in range(2):
                S.op("dve", lambda v: v.bn_stats(out=st6[i2][:, hf, :], in_=h1[i3][:, hf * 512:(hf + 1) * 512]), [B_h1[i3]], [B_st[i2]])
            S.op("dve", lambda v: v.bn_aggr(out=mv[i2][:], in_=st6[i2][:].rearrange("p a b -> p (a b)")), [B_st[i2]], [B_st[i2]])
            TS("dve", rs[i2][:], mv[i2][:, 1:2], LN_EPS, None, ALU.add, ALU.bypass, [B_st[i2]], [B_st[i2]])
            TT("pool", rs[i2][:], rs[i2][:], mhalf[:], ALU.pow, [B_st[i2], B_mh], [B_st[i2]])
            STT(nb[i2][:], mv[i2][:, 0:1], -1.0, rs[i2][:], ALU.mult, ALU.mult, [B_st[i2]], [B_st[i2]])
            ACT(h1[i3][:], h1[i3][:], AF.Identity, [B_h1[i3], B_st[i2]], [B_h1[i3]], scale=rs[i2][:, 0:1], bias=nb[i2][:, 0:1])
            TT("dve", h1[i3][:], h1[i3][:], lng[:], ALU.mult, [B_h1[i3], B_ln], [B_h1[i3]])
            TT("pool", h1[i3][:], h1[i3][:], lnb[:], ALU.add, [B_h1[i3], B_ln], [B_h1[i3]])
            if stage == 1:
                S.dma("sp", lambda g: g.dma_start(out=out_d[tt * 128:(tt + 1) * 128, :], in_=h1[i3][:]), [B_h1[i3]], [], is_output=True)
                return
            S.dma("sp", lambda g: g.dma_start(out=x1_d[tt * 128:(tt + 1) * 128, :], in_=h1[i3][:]), [B_h1[i3]], [B_x1d[tt]])
            CP("act", x1b_all[:, tt, :], h1[i3][:], [B_h1[i3]], [B_x1b[tt]])

        PSb4 = PS[:, 4, :].bitcast(BF16)
        PSb5 = PS[:, 5, :].bitcast(BF16)

        def stageB0(tt):
            TT("dve", xlo[tt % 2][:], h1[tt % NH][:], x1b_all[:, tt, :], ALU.subtract, [B_h1[tt % NH], B_x1b[tt]], [B_xlo[tt % 2]])

        def stageB1(tt):
            i2 = tt % 2
            for kc in range(8):
                S.op("pe", lambda t: t.transpose(PSb4[:, kc * 128:(kc + 1) * 128], x1b_all[:, tt, kc * 128:(kc + 1) * 128], ident_b),
                     [B_x1b[tt], B_cb], [BP[4]], signal=(kc == 7))
            for kc in range(8):
                S.op("pe", lambda t: t.transpose(PSb5[:, kc * 128:(kc + 1) * 128], xlo[i2][:, kc * 128:(kc + 1) * 128], ident_b),
                     [B_xlo[i2], B_cb], [BP[5]], signal=(kc == 7))
            CP("act", x1Th[:].rearrange("p a b -> p (a b)"), PSb4, [BP[4]], [B_x1T])
            CP("act", x1Tl[:].rearrange("p a b -> p (a b)"), PSb5, [BP[5]], [B_x1T])

        def stageB2(tt):
            bl = 6 + tt % 2
            for kc in range(8):
                MM(PS[:, bl, 0:NE], x1Th[:, kc, :], wrh[:, kc, :], kc == 0, False, [B_x1T, B_wrs], [BP[bl]])
                MM(PS[:, bl, 0:NE], x1Th[:, kc, :], wrl[:, kc, :], False, False, [B_x1T, B_wrs], [BP[bl]])
                MM(PS[:, bl, 0:NE], x1Tl[:, kc, :], wrh[:, kc, :], False, kc == 7, [B_x1T, B_wrs], [BP[bl]])
            TT("dve", lgt_all[:, tt, :], PS[:, bl, 0:NE], brt[:], ALU.add, [BP[bl], B_wr], [B_lg[tt]])
            S.op("dve", lambda v: v.max(out=top8_all[:, tt, :], in_=lgt_all[:, tt, :]), [B_lg[tt]], [B_lg[tt]])
            S.op("dve", lambda v: v.max_index(out=idx8_all[:, tt, :], in_max=top8_all[:, tt, :], in_values=lgt_all[:, tt, :]), [B_lg[tt]], [B_lg[tt]])

        for tt in range(LA):
            stageA(tt)
        if stage != 1:
            stageB0(0)
        for tt in range(16):
            if stage != 1:
                if tt + 1 < 16:
                    stageB0(tt + 1)
                stageB1(tt)
            if tt + LA < 16:
                stageA(tt + LA)
            if stage != 1:
                stageB2(tt)
        if stage == 1:
            S.barrier()
            S.finish()
            return nc

        S.barrier()
        st1c.ptr = mark_1c
        bgu = sb(st2, "bgu", [128, NE, 16], F32)
        bdn = sb(st2, "bdn", [NE, D], BF16)
        selb = sb(st2, "selb", [NE, NE * 128], BF16)
        B_bias = Buf("bias")
        LD("sp", bgu[:], bgu_d, [B_bias])
        LD("pool", bdn[:], bdn_d, [B_bias])
        for i in range(2):
            LD("pool", selb[:, i * 2048:(i + 1) * 2048], sel_d[:, i * 2048:(i + 1) * 2048], [B_bias])
        zrow = sb(st2, "zrow", [128, D], F32)
        B_z = Buf("z")
        S.op("pool", lambda g: g.memset(zrow[:], 0.0), [], [B_z])
        B_eo = [Buf(f"eo{e}") for e in range(NE + 1)]
        S.dma("sp", lambda g: g.dma_start(out=eo_d[NSLOT:NSLOT + 128, :], in_=zrow[:]), [B_z], [B_eo[NE]])
        ekf = sb(st1c, "ekf", [128, 16, 4], F32)
        tmp4 = sb(st1c, "tmp4", [128, 16, 4], F32)
        den = sb(st1c, "den", [128, 16], F32)
        mskb = sb(st1c, "mskb", [128, 16, NE], BF16)
        posf = sb(st1c, "posf", [128, 16, NE], F32)
        ohk = sb(st1c, "ohk", [128, 16, NE], F32)
        posk = sb(st1c, "posk", [128, 16, 4], F32)
        dstf = sb(st1c, "dstf", [128, 64], F32)
        ovf = sb(st1c, "ovf", [128, 64], F32)
        B_rt = Buf("router")
        B_wk = Buf("wk")
        CP("dve", ekf[:], idx8_all[:, :, 0:4], B_lg, [B_rt])
        TT("dve", tmp4[:], top8_all[:, :, 0:4], top8_all[:, :, 0:1].broadcast_to([128, 16, 4]), ALU.subtract, B_lg, [B_rt])
        ACT(wk[:], tmp4[:], AF.Exp, [B_rt], [B_wk])
        S.op("dve", lambda v: v.reduce_sum(out=den[:], in_=wk[:], axis=AX.X), [B_wk], [B_rt])
        S.op("dve", lambda v: v.reciprocal(out=den[:], in_=den[:]), [B_rt], [B_rt])
        TT("dve", wk[:], wk[:], den[:].unsqueeze(2).broadcast_to([128, 16, 4]), ALU.mult, [B_wk, B_rt], [B_wk])
        TT("dve", mskb[:], lgt_all[:], top8_all[:, :, 3:4].broadcast_to([128, 16, NE]), ALU.is_ge, B_lg, [B_rt])
        MM(PS[:, 0, :], tri_b, mskb[:].rearrange("p t e -> p (t e)"), True, False, [B_cb, B_rt], [BP[0]], sig=False)
        for t_ in range(15):
            n_ = 15 - t_
            MM(PS[:, 0, (t_ + 1) * NE:16 * NE].rearrange("p (t e) -> p t e", e=NE), ones_b, mskb[:, t_:t_ + 1, :].broadcast_to([128, n_, NE]),
               False, t_ == 14, [B_cb, B_rt], [BP[0]], sig=(t_ == 14))
        CP("act", posf[:].rearrange("p t e -> p (t e)"), PS[:, 0, :], [BP[0]], [B_rt])
        iota_b = iota_f.unsqueeze(1).broadcast_to([128, 16, NE])
        for k in range(4):
            TT("dve", ohk[:], iota_b, ekf[:, :, k:k + 1].broadcast_to([128, 16, NE]), ALU.is_equal, [B_c, B_rt], [B_rt])
            TT("dve", ohk[:], ohk[:], posf[:], ALU.mult, [B_rt], [B_rt])
            S.op("dve", lambda v: v.reduce_sum(out=posk[:, :, k], in_=ohk[:], axis=AX.X), [B_rt], [B_rt])
        poskf = posk[:].rearrange("p t k -> p (t k)")
        ekff = ekf[:].rearrange("p t k -> p (t k)")
        TS("dve", ovf[:], poskf, float(CAP), None, ALU.is_ge, ALU.bypass, [B_rt], [B_rt])
        STT(dstf[:], ekff, float(CAP), poskf, ALU.mult, ALU.add, [B_rt], [B_rt])
        STT(dstf[:], ovf[:], float(4 * NSLOT), dstf[:], ALU.mult, ALU.add, [B_rt], [B_rt])
        B_dst = Buf("dst")
        CP("dve", dsts2[:], dstf[:], [B_rt], [B_dst])
        TS("dve", dstf[:], dstf[:], float(NSLOT), None, ALU.min, ALU.bypass, [B_rt], [B_rt])
        CP("dve", dstg2[:], dstf[:], [B_rt], [B_dst])
        for tt in range(16):
            B_route[tt] = B_dst
        B_route_w = B_wk
        R2w = Region(6, 102)
        wgu0 = sb(R2w, "wgu0", [128, 8, 2 * D], BF16)
        wdn0 = sb(R2w, "wdn0", [128, 8, D], BF16)
        wdn1 = sb(R2w, "wdn1", [128, 8, D], BF16)
        wgu1 = sb(R2w, "wgu1", [128, 8, 2 * D], BF16)
        wgu = [wgu0, wgu1]
        wdn = [wdn0, wdn1]
        B_wgu = [Buf("wgu0"), Buf("wgu1")]
        B_wdn = [Buf("wdn0"), Buf("wdn1")]
        LD("pool", wgu[0][:], kp(w_gu_d[0]), [B_wgu[0]])
        LD("pool", wdn[0][:], kp(w_dn_d[0]), [B_wdn[0]])
        B_xgs = [Buf(f"xgs{i}") for i in range(64)]
        for tt in range(16):
            for k in range(4):
                S.dma("pool", lambda g: g.indirect_dma_start(out=xg_d, out_offset=bass.IndirectOffsetOnAxis(ap=dsts2[:, tt * 4 + k:tt * 4 + k + 1], axis=0),
                                                              in_=x1b_all[:, tt, :], in_offset=None, bounds_check=reg_sc, oob_is_err=False),
                      [B_x1b[tt], B_dst, B_xg], [B_xgs[tt * 4 + k]])
        S.barrier()

        xgt = [sb(st2, f"xgt{i}", [128, 3, D], BF16) for i in range(3)]
        B_xgt = [Buf("xgt0"), Buf("xgt1"), Buf("xgt2")]
        xgT = [sb(st2, f"xgT{i}", [128, 8, CAP], BF16) for i in range(2)]
        B_xgT = [Buf("xgT0"), Buf("xgT1")]
        hidT = [sb(st2, f"hidT{i}", [128, 8, CAP], BF16) for i in range(2)]
        B_hid = [[Buf(f"hid{i}_{m}") for m in range(8)] for i in range(2)]
        gt = [sb(st2, f"gt{i}", [128, CAP], F32) for i in range(2)]
        sg = [sb(st2, f"sg{i}", [128, CAP], F32) for i in range(2)]
        ut = [sb(st2, f"ut{i}", [128, CAP], F32) for i in range(2)]
        B_gt = [Buf("gt0"), Buf("gt1")]
        B_sg = [Buf("sg0"), Buf("sg1")]
        B_ut = [Buf("ut0"), Buf("ut1")]
        eot = [sb(st2, f"eot{i}", [128, D], F32) for i in range(2)]
        B_eot = [Buf("eot0"), Buf("eot1")]
        PSb = [PS[:, 6, :].bitcast(BF16), PS[:, 7, :].bitcast(BF16)]

        def load_expert(e):
            i = e % 2
            if e == 0:
                return
            LD("pool", wgu[i][:], kp(w_gu_d[e]), [B_wgu[i]])
            LD("pool", wdn[i][:], kp(w_dn_d[e]), [B_wdn[i]])

        def load_tokens(e):
            LD("sp", xgt[e % 3][:], xg_d[e * CAP:(e + 1) * CAP, :].rearrange("(a p) n -> p a n", p=128), [B_xgt[e % 3]], R=B_xgs)

        def transposes(e, part=None):
            i = e % 2
            xg_, Bxg_ = xgt[e % 3], B_xgt[e % 3]
            parts = range(6) if part is None else [part]
            for pt in parts:
                a = pt // 2
                pb_ = a % 2
                for kc in range(4 * (pt % 2), 4 * (pt % 2) + 4):
                    S.op("pe", lambda t: t.transpose(PSb[pb_][:, kc * 128:(kc + 1) * 128], xg_[:, a, kc * 128:(kc + 1) * 128], ident_b),
                         [Bxg_, B_cb], [BP[6 + pb_]], signal=(kc == 7))
                if pt % 2 == 1:
                    CP("act", xgT[i][:, :, a * 128:(a + 1) * 128], PSb[pb_].rearrange("p (k s) -> p k s", s=128), [BP[6 + pb_]], [B_xgT[i]])

        mctr = [0]

        def gate_up(e):
            i = e % 2
            pend = None

            def finish(m, j2):
                STT(hidT[i][:, m, :], ut[j2][:], 1.0, sg[j2][:], ALU.add, ALU.mult, [B_sg[j2], B_ut[j2]], [B_hid[i][m]])

            for m in range(8):
                j2 = mctr[0] % 2
                bg = 2 * j2
                bu = bg + 1
                mctr[0] += 1
                for kc in range(8):
                    MM(PS[:, bg, 0:CAP], wgu[i][:, kc, m * 128:(m + 1) * 128], xgT[i][:, kc, :], kc == 0, kc == 7, [B_wgu[i], B_xgT[i]], [BP[bg]])
                for kc in range(8):
                    MM(PS[:, bu, 0:CAP], wgu[i][:, kc, D + m * 128:D + (m + 1) * 128], xgT[i][:, kc, :], kc == 0, kc == 7, [B_wgu[i], B_xgT[i]], [BP[bu]])
                if m >= 2 and e + 1 < NE:
                    transposes(e + 1, part=m - 2)
                TS("dve", gt[j2][:], PS[:, bg, 0:CAP], bgu[:, e, m:m + 1], 7.0, ALU.add, ALU.min, [BP[bg], B_bias], [B_gt[j2]])
                ACT(sg[j2][:], gt[j2][:], AF.Gelu_apprx_sigmoid, [B_gt[j2]], [B_sg[j2]])
                TS("dve", ut[j2][:], PS[:, bu, 0:CAP], bgu[:, e, 8 + m:9 + m], None, ALU.add, ALU.bypass, [BP[bu], B_bias], [B_ut[j2]])
                TS("pool", ut[j2][:], ut[j2][:], 7.0, -7.0, ALU.min, ALU.max, [B_ut[j2]], [B_ut[j2]])
                if pend is not None:
                    finish(*pend)
                pend = (m, j2)
            finish(*pend)

        ectr = [0]

        def down(e):
            i = e % 2
            for a in range(3):
                j2 = ectr[0] % 2
                ectr[0] += 1
                b0 = 4 if a % 2 == 0 else 6
                for hf in range(2):
                    bk = b0 + hf
                    for kc in range(8):
                        MM(PS[:, bk, :], hidT[i][:, kc, a * 128:(a + 1) * 128], wdn[i][:, kc, hf * 512:(hf + 1) * 512], kc == 0, False, [B_hid[i][kc], B_wdn[i]], [BP[bk]])
                    MM(PS[:, bk, :], selb[:, e * 128:(e + 1) * 128], bdn[:, hf * 512:(hf + 1) * 512], False, True, [B_bias], [BP[bk]])
                CP("act", eot[j2][:], PS[:, b0:b0 + 2, :].rearrange("p a b -> p (a b)"), [BP[b0], BP[b0 + 1]], [B_eot[j2]])
                S.dma("sp", lambda g: g.dma_start(out=eo_d[e * CAP + a * 128:e * CAP + (a + 1) * 128, :], in_=eot[j2][:]), [B_eot[j2]], [B_eo[e]])

        load_tokens(0)
        load_tokens(1)
        load_expert(0)
        transposes(0)
        for e in range(NE):
            if e + 1 < NE:
                load_expert(e + 1)
            if e + 2 < NE:
                load_tokens(e + 2)
            gate_up(e)
            down(e)
        S.barrier()

        l2g = sb(st3, "l2g", [128, D], F32)
        l2b = sb(st3, "l2b", [128, D], F32)
        B_l2 = Buf("ln2")
        LD("sp", l2g[:], lnp_d[2], [B_l2])
        LD("sp", l2b[:], lnp_d[3], [B_l2])
        NG = 3
        gat = [[sb(st3, f"gat{i}_{k}", [128, D], F32) for k in range(4)] for i in range(NG)]
        B_gat = [[Buf(f"gat{i}_{k}") for k in range(4)] for i in range(NG)]
        x1t = [sb(st3, f"x1t{i}", [128, D], F32) for i in range(NG)]
        B_x1t = [Buf(f"x1t{i}") for i in range(NG)]
        st6b = [sb(st3, f"st6b{i}", [128, 2, 6], F32) for i in range(2)]
        mvb = [sb(st3, f"mvb{i}", [128, 2], F32) for i in range(2)]
        rsb = [sb(st3, f"rsb{i}", [128, 1], F32) for i in range(2)]
        nbb = [sb(st3, f"nbb{i}", [128, 1], F32) for i in range(2)]
        B_stb = [Buf("stb0"), Buf("stb1")]

        def fetch(tt):
            i3 = tt % NG
            LD("sp", x1t[i3][:], x1_d[tt * 128:(tt + 1) * 128, :], [B_x1t[i3]], R=[B_x1d[tt]])
            for k in range(4):
                S.dma("pool", lambda g: g.indirect_dma_start(out=gat[i3][k][:], out_offset=None, in_=eo_d,
                                                              in_offset=bass.IndirectOffsetOnAxis(ap=dstg2[:, tt * 4 + k:tt * 4 + k + 1], axis=0),
                                                              bounds_check=reg_ga, oob_is_err=False),
                      B_eo + [B_route[tt]], [B_gat[i3][k]])

        fetch(0)
        fetch(1)
        for tt in range(16):
            i3 = tt % NG
            i2 = tt % 2
            if tt + 2 < 16:
                fetch(tt + 2)
            acc = x1t[i3]
            Ba = B_x1t[i3]
            ACT(acc[:], acc[:], AF.Copy, [Ba], [Ba], scale=ALPHA)
            for k in range(4):
                STT(acc[:], gat[i3][k][:], wk[:, tt, k:k + 1], acc[:], ALU.mult, ALU.add, [B_gat[i3][k], B_route[tt], Ba], [Ba])
            for hf in range(2):
                S.op("dve", lambda v: v.bn_stats(out=st6b[i2][:, hf, :], in_=acc[:, hf * 512:(hf + 1) * 512]), [Ba], [B_stb[i2]])
            S.op("dve", lambda v: v.bn_aggr(out=mvb[i2][:], in_=st6b[i2][:].rearrange("p a b -> p (a b)")), [B_stb[i2]], [B_stb[i2]])
            ACT(rsb[i2][:], mvb[i2][:, 1:2], AF.Sqrt, [B_stb[i2]], [B_stb[i2]], scale=1.0, bias=LN_EPS)
            S.op("dve", lambda v: v.reciprocal(out=rsb[i2][:], in_=rsb[i2][:]), [B_stb[i2]], [B_stb[i2]])
            STT(nbb[i2][:], mvb[i2][:, 0:1], -1.0, rsb[i2][:], ALU.mult, ALU.mult, [B_stb[i2]], [B_stb[i2]])
            ACT(acc[:], acc[:], AF.Identity, [Ba, B_stb[i2]], [Ba], scale=rsb[i2][:, 0:1], bias=nbb[i2][:, 0:1])
            TT("dve", acc[:], acc[:], l2g[:], ALU.mult, [Ba, B_l2], [Ba])
            TT("pool", acc[:], acc[:], l2b[:], ALU.add, [Ba, B_l2], [Ba])
            S.dma("sp", lambda g: g.dma_start(out=out_d[tt * 128:(tt + 1) * 128, :], in_=acc[:]), [Ba], [], is_output=True)
        S.finish()
    return nc


def _consts(h):
    c = np.zeros((128, C_END), np.float32)
    c[:, C_ID:C_ID + 128] = np.eye(128, dtype=np.float32)
    c[:, C_ONE:C_ONE + 128] = 1.0
    k = np.arange(128)[:, None] // 64
    q = np.arange(128)[None, :] // 64
    c[:, C_MOWN:C_MOWN + 128] = (k <= q)
    c[:, C_MOTH:C_MOTH + 128] = (k <= q) if h == 1 else (k < q)
    c[:, C_TRI:C_TRI + 128] = (np.arange(128)[:, None] < np.arange(128)[None, :])
    c[:, C_IOTA:C_IOTA + 32] = np.arange(32, dtype=np.float32)[None, :]
    inv_freq = (10000.0 ** (-np.arange(0, 64, 2, dtype=np.float32) / np.float32(64))).astype(np.float32)
    c[0:64, C_INVF] = np.tile(inv_freq.astype(np.float64) / (2 * np.pi), 2).astype(np.float32)
    c[0:32, C_SGN] = -1.0
    c[32:64, C_SGN] = 1.0
    return c


def _prep_shared(inp):
    f = lambda a: np.ascontiguousarray(a, dtype=np.float32)
    w_in = inp["w_in"][0]
    o = np.cumsum([0, 1024, 1024, 1024, 384, 256, 64, 1024, 1024])
    gb, gc, hc, ql, kv, kpe, gcv, gat = [w_in[:, o[i]:o[i + 1]] for i in range(8)]
    sw = np.concatenate([np.arange(32, 64), np.arange(0, 32)])
    sh = {}
    sh["w_inA"] = f(np.concatenate([np.concatenate([gb[:, i * 128:(i + 1) * 128], gc[:, i * 128:(i + 1) * 128], hc[:, i * 128:(i + 1) * 128]], 1) for i in range(8)], 1))
    sh["w_inQ"] = f(ql)
    sh["w_inK"] = f(np.concatenate([kv, kpe, kpe[:, sw]], 1))
    wcb, wab = inp["w_conv_branch"][0], inp["w_attn_branch"][0]
    sh["w_mrg"] = f(np.concatenate([np.concatenate([wcb[:, i * 128:(i + 1) * 128], wab[:, i * 128:(i + 1) * 128], gcv[:, i * 128:(i + 1) * 128], gat[:, i * 128:(i + 1) * 128]], 1) for i in range(8)], 1))
    wuq = inp["w_uq"][0].reshape(384, 8, 192)
    sh["w_uq"] = f(np.concatenate([wuq[:, :, 0:128].reshape(384, 1024), wuq[:, :, 128:192].reshape(384, 512), wuq[:, :, 128:192][:, :, sw].reshape(384, 512)], 1))
    sh["w_uk"] = f(inp["w_uk"][0])
    sh["w_uv"] = f(inp["w_uv"][0])
    sh["w_out"] = f(inp["w_out"][0])
    sh["w_gu"] = f(inp["w_gate_up"][0])
    sh["w_dn"] = f(inp["w_down"][0])
    sh["cw"] = f(inp["conv_w"][0].T.reshape(8, 128, 3).transpose(1, 0, 2))
    sh["gq"] = f(inp["q_norm_g"][0].reshape(3, 128).T)
    sh["gkv"] = f(inp["kv_norm_g"][0].reshape(2, 128).T)
    sh["lnp"] = f(np.stack([np.broadcast_to(inp[k][0][None, :], (128, D)) for k in ("ln1_g", "ln1_b", "ln2_g", "ln2_b")]))
    sh["w_r"] = f(inp["w_router"][0].reshape(8, 128, NE).transpose(1, 0, 2))
    sh["b_r"] = f(np.broadcast_to(inp["b_router"][0][None, :], (128, NE)))
    sh["b_gu"] = f(inp["b_gate_up"][0].reshape(NE, 16, 128).transpose(2, 0, 1))
    sh["b_dn"] = f(inp["b_down"][0])
    sel = np.zeros((NE, NE, 128), np.float32)
    sel[np.arange(NE), np.arange(NE), :] = 1.0
    sh["selc"] = f(sel.reshape(NE, NE * 128))
    return sh


def _own_tokens(h):
    ch = 2 * np.arange(32) + h
    return (ch[:, None] * 64 + np.arange(64)[None, :]).ravel()


def _prep_core(inp, c):
    b, h = c // 2, c % 2
    xb = np.asarray(inp["x"][b], dtype=np.float32)
    own, oth = _own_tokens(h), _own_tokens(1 - h)
    halo = ((2 * np.arange(32) + h)[:, None] * 64 + np.array([-2, -1])[None, :]).ravel()
    xh = xb[np.maximum(halo, 0)].copy()
    xh[halo < 0] = 0.0
    p = np.asarray(inp["positions"][b]).astype(np.int32)
    d = {
        "xT_own": np.ascontiguousarray(xb[own].T),
        "xT_oth": np.ascontiguousarray(xb[oth].T),
        "xT_halo": np.ascontiguousarray(xh.T),
        "x_own": np.ascontiguousarray(xb[own]),
        "pos": np.ascontiguousarray(np.broadcast_to(np.concatenate([p[own], p[oth]])[None, :], (64, 2 * T))).astype(np.int32),
        "cst": _consts(h),
    }
    return d


_NC_CACHE = {}


def kernel(**inputs):
    inputs = {k: np.asarray(v) for k, v in inputs.items()}
    if "prog" not in _NC_CACHE:
        _NC_CACHE["prog"] = build_program()
    nc = _NC_CACHE["prog"]
    sh = _prep_shared(inputs)
    in_maps = []
    for c in range(NCORES):
        d = _prep_core(inputs, c)
        d.update(sh)
        in_maps.append(d)
    res = run_bass_kernel_spmd(nc, in_maps, core_ids=list(range(NCORES)))
    out = np.zeros((4, 4096, D), np.float32)
    for c in range(NCORES):
        out[c // 2, _own_tokens(c % 2), :] = res.results[c]["out"]
    return out
```
